# Optimizing a Trainium2 kernel written in Bass

```python
import jax
import jax.numpy as jnp
from jax import lax
import numpy as np

D_MODEL = 1024
BATCH = 4
SEQ = 8192
DEPTH = 2

CTX_LEN = 256
GRID_W = 64

POOL_WINDOWS = (2, 4, 8, 16)
POOL_WIDTH = 256
POOL_GROUP = POOL_WIDTH // len(POOL_WINDOWS)
SGU_WIDTH = 256
SGU_GROUPS = 4
SGU_GROUP = SGU_WIDTH // SGU_GROUPS
CHUNK = 128
MLA_HEADS = 8
QK_NOPE = 64
QK_ROPE = 32
V_HEAD = 64
Q_LORA = 384
KV_LORA = 256
MLA_WIDTH = MLA_HEADS * V_HEAD
QK_HEAD = QK_NOPE + QK_ROPE
SM_SCALE = QK_HEAD ** -0.5
Q_BLOCK = 128
ROPE_BASE = 10000.0
ROPE_AXIS = QK_ROPE // 2
ROPE_PAIRS = ROPE_AXIS // 2
N_BRANCH = 3
OFF_U = POOL_WIDTH
OFF_V = OFF_U + SGU_WIDTH
OFF_QA = OFF_V + SGU_WIDTH
OFF_KVA = OFF_QA + Q_LORA
OFF_KR = OFF_KVA + KV_LORA
OFF_GATE = OFF_KR + QK_ROPE
IN_WIDTH = OFF_GATE + N_BRANCH * D_MODEL
N_EXPERTS = 16
N_GROUPS = 4
EXPERTS_PER_GROUP = N_EXPERTS // N_GROUPS
TOP_K = 2
D_EXPERT = 512
EPS = 1e-6

kernel_name = "hybrid_pool_sgu_mla_moe_diffusion_trunk"


def rmsnorm(x, g):
    xf = x.astype(jnp.float32)
    y = xf * lax.rsqrt(jnp.mean(xf * xf, axis=-1, keepdims=True) + EPS)
    return (y * g.astype(jnp.float32)).astype(x.dtype)


def adaln(cond, w_mod, b_mod):
    m = jax.nn.silu(cond) @ w_mod + b_mod
    return [a[:, None, :] for a in jnp.split(m, 6, axis=-1)]


def modulate(h, shift, scale):
    return h * (1 + scale) + shift


def axial_rope_tables(n):
    rows = n // GRID_W
    row = jnp.repeat(jnp.arange(rows, dtype=jnp.float32), GRID_W)
    col = jnp.tile(jnp.arange(GRID_W, dtype=jnp.float32), rows)
    inv = ROPE_BASE ** (-(jnp.arange(ROPE_PAIRS, dtype=jnp.float32) * 2.0 / ROPE_AXIS))
    ang_r = row[:, None] * inv
    ang_c = col[:, None] * inv
    ang = jnp.concatenate([ang_r, ang_r, ang_c, ang_c], axis=-1)
    return jnp.cos(ang), jnp.sin(ang)


def apply_rope(x, cos, sin):
    x4 = x.reshape(x.shape[:-1] + (4, ROPE_PAIRS))
    rot = jnp.stack([-x4[..., 1, :], x4[..., 0, :], -x4[..., 3, :], x4[..., 2, :]], axis=-2).reshape(x.shape)
    return (x.astype(jnp.float32) * cos + rot.astype(jnp.float32) * sin).astype(x.dtype)


def split_proj(proj):
    return jnp.split(proj, [OFF_U, OFF_V, OFF_QA, OFF_KVA, OFF_KR, OFF_GATE], axis=-1)


def pool_mixer(p, pool_w, pool_scale):
    bsz, n, _ = p.shape
    pf = p.astype(jnp.float32)
    cs = jnp.concatenate([jnp.zeros_like(pf[:, :1]), jnp.cumsum(pf, axis=1)], axis=1)
    t = jnp.arange(n)
    outs = []
    for gi, w in enumerate(POOL_WINDOWS):
        lo = jnp.clip(t - w // 2, 0, n)
        hi = jnp.clip(t + (w - w // 2), 0, n)
        csg = cs[..., gi * POOL_GROUP:(gi + 1) * POOL_GROUP]
        cnt = (hi - lo).astype(jnp.float32)[None, :, None]
        mean = (jnp.take(csg, hi, axis=1) - jnp.take(csg, lo, axis=1)) / cnt
        outs.append(mean - pf[..., gi * POOL_GROUP:(gi + 1) * POOL_GROUP])
    d = jnp.stack(outs, axis=2).astype(p.dtype)
    y = jnp.einsum('bngc,gcd->bngd', d, pool_w).reshape(bsz, n, POOL_WIDTH)
    return y * pool_scale


def sgu_mixer(u, v, norm_g, ws, b):
    bsz, n, _ = v.shape
    u = jax.nn.gelu(u)
    v = rmsnorm(jax.nn.gelu(v), norm_g)
    vc = v.reshape(bsz, n // CHUNK, CHUNK, SGU_GROUPS, SGU_GROUP)
    mixed = jnp.einsum('gij,bnjgc->bnigc', ws, vc) + b.T[:, :, None]
    return u * mixed.reshape(bsz, n, SGU_WIDTH)


def mla_q(q_a, lp, rope_tabs):
    bsz, n, _ = q_a.shape
    q = (rmsnorm(q_a, lp['qa_norm_g']) @ lp['w_uq']).reshape(bsz, n, MLA_HEADS, QK_HEAD)
    q_nope = rmsnorm(q[..., :QK_NOPE], lp['q_norm_g'][:QK_NOPE])
    q_rope = rmsnorm(q[..., QK_NOPE:], lp['q_norm_g'][QK_NOPE:])
    if rope_tabs is not None:
        cos, sin = rope_tabs
        q_rope = apply_rope(q_rope, cos[:, None, :], sin[:, None, :])
    return q_nope, q_rope


def mla_kv(kv_a, k_r, lp, rope_tabs):
    bsz, n, _ = kv_a.shape
    kv = (rmsnorm(kv_a, lp['kva_norm_g']) @ lp['w_ukv']).reshape(bsz, n, MLA_HEADS, QK_NOPE + V_HEAD)
    k_nope = rmsnorm(kv[..., :QK_NOPE], lp['k_norm_g'][:QK_NOPE])
    v = kv[..., QK_NOPE:]
    k_rope = rmsnorm(k_r, lp['k_norm_g'][QK_NOPE:])
    if rope_tabs is not None:
        cos, sin = rope_tabs
        k_rope = apply_rope(k_rope, cos, sin)
    return (k_nope, k_rope, v)


def attend(q_nope, q_rope, k_nope, k_rope, v):
    bsz, n, heads, _ = q_nope.shape
    nb = n // Q_BLOCK

    def blocks(a):
        return a.reshape((bsz, nb, Q_BLOCK) + a.shape[2:]).swapaxes(0, 1)

    def one(args):
        qn, qr = args
        s = jnp.einsum('bqhd,bkhd->bhqk', qn, k_nope) + jnp.einsum('bqhr,bkr->bhqk', qr, k_rope)
        p = jax.nn.softmax(s.astype(jnp.float32) * SM_SCALE, axis=-1).astype(v.dtype)
        return jnp.einsum('bhqk,bkhd->bqhd', p, v)

    o = lax.map(one, (blocks(q_nope), blocks(q_rope)))
    return o.swapaxes(0, 1).reshape(bsz, n, heads * V_HEAD)


def hybrid_mixer(p, u, v, q_a, g, keys, lp, rope_tabs):
    y_pool = pool_mixer(p, lp['pool_w'], lp['pool_scale'])
    y_sgu = sgu_mixer(u, v, lp['sgu_norm_g'], lp['sgu_ws'], lp['sgu_b'])
    q_nope, q_rope = mla_q(q_a, lp, rope_tabs)
    y_att = attend(q_nope, q_rope, *keys)
    gp, gs, ga = jnp.split(jax.nn.sigmoid(g + lp['b_gate']), N_BRANCH, axis=-1)
    merged = (gp * (y_pool @ lp['w_br_pool'])
              + gs * (y_sgu @ lp['w_br_sgu'])
              + ga * (y_att @ lp['w_br_mla']))
    return merged @ lp['w_out']


def moe(h, router_w, router_b, w_gate, w_up, w_down):
    shp = h.shape
    t = h.reshape(-1, shp[-1])
    scores = jax.nn.sigmoid((t @ router_w).astype(jnp.float32))
    sel = scores + router_b.astype(jnp.float32)
    grp_score = lax.top_k(sel.reshape(-1, N_GROUPS, EXPERTS_PER_GROUP), 2)[0].sum(-1)
    best = jnp.argmax(grp_score, axis=-1)
    in_grp = (jnp.arange(N_EXPERTS) // EXPERTS_PER_GROUP)[None, :] == best[:, None]
    _, idx = lax.top_k(jnp.where(in_grp, sel, -jnp.inf), TOP_K)
    w = jnp.take_along_axis(scores, idx, axis=-1)
    w = w / jnp.sum(w, axis=-1, keepdims=True)
    gate = jnp.sum(jax.nn.one_hot(idx, N_EXPERTS, dtype=jnp.float32) * w[..., None], axis=-2).astype(t.dtype)
    y = jnp.zeros_like(t)
    for e in range(N_EXPERTS):
        a = jax.nn.silu(t @ w_gate[e]) * (t @ w_up[e])
        y = y + gate[:, e:e + 1] * (a @ w_down[e])
    return y.reshape(shp)


def setup_inputs(seed: int = 0) -> dict:
    key = jax.random.key(seed)
    ks = jax.random.split(key, 32)
    f32 = jnp.float32
    L, D = DEPTH, D_MODEL

    def nrm(k, shape, scale):
        return jax.random.normal(k, shape, f32) * scale

    def gain(k, shape):
        return 1.0 + 0.05 * jax.random.normal(k, shape, f32)

    return {
        "x": nrm(ks[0], (BATCH, SEQ, D), 1.0),
        "c": nrm(ks[1], (BATCH, D), 1.0),
        "ctx": nrm(ks[2], (BATCH, CTX_LEN, D), 1.0),
        "c_ctx": nrm(ks[3], (D,), 1.0),
        "w_mod": nrm(ks[4], (L, D, 6 * D), 0.5 * D ** -0.5),
        "b_mod": nrm(ks[5], (L, 6 * D), 0.02),
        "norm1_g": gain(ks[6], (L, D)),
        "norm2_g": gain(ks[7], (L, D)),
        "w_in": nrm(ks[8], (L, D, IN_WIDTH), D ** -0.5),
        "pool_w": nrm(ks[9], (L, len(POOL_WINDOWS), POOL_GROUP, POOL_GROUP), POOL_GROUP ** -0.5),
        "pool_scale": gain(ks[10], (L, POOL_WIDTH)),
        "sgu_norm_g": gain(ks[11], (L, SGU_WIDTH)),
        "sgu_ws": nrm(ks[12], (L, SGU_GROUPS, CHUNK, CHUNK), CHUNK ** -0.5),
        "sgu_b": gain(ks[13], (L, SGU_GROUPS, CHUNK)),
        "qa_norm_g": gain(ks[14], (L, Q_LORA)),
        "w_uq": nrm(ks[15], (L, Q_LORA, MLA_HEADS * QK_HEAD), Q_LORA ** -0.5),
        "kva_norm_g": gain(ks[16], (L, KV_LORA)),
        "w_ukv": nrm(ks[17], (L, KV_LORA, MLA_HEADS * (QK_NOPE + V_HEAD)), KV_LORA ** -0.5),
        "q_norm_g": gain(ks[18], (L, QK_HEAD)),
        "k_norm_g": gain(ks[19], (L, QK_HEAD)),
        "w_br_pool": nrm(ks[20], (L, POOL_WIDTH, D), POOL_WIDTH ** -0.5),
        "w_br_sgu": nrm(ks[21], (L, SGU_WIDTH, D), SGU_WIDTH ** -0.5),
        "w_br_mla": nrm(ks[22], (L, MLA_WIDTH, D), MLA_WIDTH ** -0.5),
        "b_gate": nrm(ks[23], (L, N_BRANCH * D), 0.02),
        "w_out": nrm(ks[24], (L, D, D), D ** -0.5),
        "router_w": nrm(ks[25], (D, N_EXPERTS), D ** -0.5),
        "router_b": nrm(ks[26], (N_EXPERTS,), 0.01),
        "w_e_gate": nrm(ks[27], (L, N_EXPERTS, D, D_EXPERT), D ** -0.5),
        "w_e_up": nrm(ks[28], (L, N_EXPERTS, D, D_EXPERT), D ** -0.5),
        "w_e_down": nrm(ks[29], (L, N_EXPERTS, D_EXPERT, D), D_EXPERT ** -0.5),
    }


def reference(x, c, ctx, c_ctx, w_mod, b_mod, norm1_g, norm2_g, w_in, pool_w, pool_scale,
              sgu_norm_g, sgu_ws, sgu_b, qa_norm_g, w_uq, kva_norm_g, w_ukv, q_norm_g, k_norm_g,
              w_br_pool, w_br_sgu, w_br_mla, b_gate, w_out, router_w, router_b,
              w_e_gate, w_e_up, w_e_down):
    n_lat = x.shape[1]
    rope_tabs = axial_rope_tables(n_lat)
    x_lat, x_ctx = x, ctx
    for l in range(DEPTH):
        last = l == DEPTH - 1
        lp = dict(w_in=w_in[l], pool_w=pool_w[l], pool_scale=pool_scale[l], sgu_norm_g=sgu_norm_g[l],
                  sgu_ws=sgu_ws[l], sgu_b=sgu_b[l], qa_norm_g=qa_norm_g[l], w_uq=w_uq[l],
                  kva_norm_g=kva_norm_g[l], w_ukv=w_ukv[l], q_norm_g=q_norm_g[l], k_norm_g=k_norm_g[l],
                  w_br_pool=w_br_pool[l], w_br_sgu=w_br_sgu[l], w_br_mla=w_br_mla[l],
                  b_gate=b_gate[l], w_out=w_out[l])
        sh1, sc1, g1, sh2, sc2, g2 = adaln(c, w_mod[l], b_mod[l])
        csh1, csc1, cg1, csh2, csc2, cg2 = adaln(c_ctx[None, :], w_mod[l], b_mod[l])

        hc = modulate(rmsnorm(x_ctx, norm1_g[l]), csh1, csc1)
        if last:
            kv_cols = hc @ lp['w_in'][:, OFF_KVA:OFF_GATE]
            kv_a_c, k_r_c = kv_cols[..., :KV_LORA], kv_cols[..., KV_LORA:]
        else:
            pc, uc, vc, qac, kv_a_c, k_r_c, gc = split_proj(hc @ lp['w_in'])
        ctx_keys = mla_kv(kv_a_c, k_r_c, lp, None)

        h = modulate(rmsnorm(x_lat, norm1_g[l]), sh1, sc1)
        p, u, v, q_a, kv_a, k_r, g = split_proj(h @ lp['w_in'])
        lat_keys = mla_kv(kv_a, k_r, lp, rope_tabs)
        keys = tuple(jnp.concatenate([a, b], axis=1) for a, b in zip(lat_keys, ctx_keys))
        x_lat = x_lat + g1 * hybrid_mixer(p, u, v, q_a, g, keys, lp, rope_tabs)
        h2 = modulate(rmsnorm(x_lat, norm2_g[l]), sh2, sc2)
        x_lat = x_lat + g2 * moe(h2, router_w, router_b, w_e_gate[l], w_e_up[l], w_e_down[l])

        if not last:
            x_ctx = x_ctx + cg1 * hybrid_mixer(pc, uc, vc, qac, gc, ctx_keys, lp, None)
            hc2 = modulate(rmsnorm(x_ctx, norm2_g[l]), csh2, csc2)
            x_ctx = x_ctx + cg2 * moe(hc2, router_w, router_b, w_e_gate[l], w_e_up[l], w_e_down[l])
    return x_lat
```

```python
import contextlib
import numpy as np
import concourse.bass as bass
import concourse.mybir as mybir
from concourse.bass_utils import run_bass_kernel_spmd

F32 = mybir.dt.float32
BF16 = mybir.dt.bfloat16
AF = mybir.ActivationFunctionType
ALU = mybir.AluOpType
AX = mybir.AxisListType

D = 1024
NLAT = 4096
NCTX = 256
NTOK = NLAT + NCTX
NT = NTOK // 128
OFF_U, OFF_V, OFF_QA, OFF_KVA, OFF_KR, OFF_GATE = 256, 512, 768, 1152, 1408, 1440
IN_W = 4512
EPS = 1e-6
SM_SCALE = 96 ** -0.5
NE = 16


class Trk:
    def __init__(self, handle, name):
        self.h = handle
        self.name = name
        self.writes = []
        self.reads = []
        self.war = []

    def __getitem__(self, idx):
        return self.h[idx]

    def ap(self):
        return self.h.ap()


def _compress(evs):
    best = {}
    for s, v in evs:
        k = id(s)
        if k not in best or best[k][1] < v:
            best[k] = (s, v)
    return list(best.values())


class Prog:
    NQ = 12
    WKEYS = ("out", "accum_out")

    def __init__(self):
        self.nc = bass.Bass("TRN2", target_bir_lowering=False)
        self.stack = contextlib.ExitStack()
        self.ops = {k: [] for k in ("tensor", "vector", "scalar", "gpsimd", "sync")}
        self.esem = {}
        self.ecount = {k: 0 for k in self.ops}
        self.known = {k: {} for k in self.ops}
        for k in self.ops:
            self.esem[k] = self.stack.enter_context(self.nc.semaphore("s_" + k))
        self.qsems, self.qn, self.quse = {}, {}, {}
        for q in ("sync", "gpsimd", "scalar"):
            self.qsems[q] = [self.stack.enter_context(self.nc.semaphore(f"d_{q}{i}")) for i in range(self.NQ)]
            self.qn[q] = 0
            self.quse[q] = [0] * self.NQ
        self.ccsem = self.stack.enter_context(self.nc.semaphore("cc_sem"))
        self.cccount = 0
        self.nt = 0
        self.out_events = []
        self.pstack = None
        self.reg = {}
        self.drams = {}
        self.ext_in = []
        self.ext_out = []

    def begin_phase(self):
        assert self.pstack is None
        self.pstack = contextlib.ExitStack()
        self.scrpool = {}

    def end_phase(self, final=False):
        nc = self.nc
        for q in self.qsems:
            for i, sem in enumerate(self.qsems[q]):
                if self.quse[q][i]:
                    self._wait("sync", (sem, self.quse[q][i] * 16))
        if self.cccount:
            self._wait("sync", (self.ccsem, self.cccount))
        if final:
            for ev in self.out_events:
                self._wait("sync", ev)
        with nc.Block() as block:
            def emit(engname):
                def body(e):
                    for o in self.ops[engname]:
                        if o[0] == "wait":
                            e.wait_ge(o[1], o[2])
                        elif o[0] == "op":
                            o[1](e).then_inc(o[2], 1)
                        elif o[0] == "cc":
                            _, in_ap, out_ap, sem, rg = o
                            e.collective_compute("AllGather", ALU.bypass, replica_groups=rg, ins=[in_ap], outs=[out_ap]).then_inc(sem, 1)
                        else:
                            _, out_ap, in_ap, sem, kw = o
                            e.dma_start(out=out_ap, in_=in_ap, **kw).then_inc(sem, 16)
                return body
            for k, regf in (("sync", block.sync), ("scalar", block.scalar), ("vector", block.vector),
                            ("gpsimd", block.gpsimd), ("tensor", block.tensor)):
                if self.ops[k]:
                    regf(emit(k))
        for k in self.ops:
            self.ops[k] = []
        self.pstack.close()
        self.pstack = None

    def finish(self):
        self.stack.close()
        return self.nc

    def dram(self, name, shape, dtype, kind):
        if name in self.drams:
            return self.drams[name]
        t = Trk(self.nc.dram_tensor(name, list(shape), dtype, kind=kind), name)
        t.kind = kind
        self.drams[name] = t
        self.reg[name] = t
        if kind == "ExternalInput":
            self.ext_in.append(name)
        elif kind == "ExternalOutput":
            self.ext_out.append(name)
        return t

    def span_begin(self):
        assert self.pstack is None and getattr(self, "sstack", None) is None
        self.sstack = contextlib.ExitStack()

    def span_end(self):
        assert self.pstack is None
        self.sstack.close()
        self.sstack = None

    def sb_span(self, name, shape, dtype=F32):
        self.nt += 1
        nm = f"{name}_{self.nt}"
        h = self.sstack.enter_context(self.nc.sbuf_tensor(nm, list(shape), dtype))
        t = Trk(h, nm)
        self.reg[nm] = t
        return t

    def _alloc(self, fn, name, shape, dtype, top):
        self.nt += 1
        nm = f"{name}_{self.nt}"
        st = self.stack if (top or self.pstack is None) else self.pstack
        h = st.enter_context(fn(nm, list(shape), dtype))
        t = Trk(h, nm)
        self.reg[nm] = t
        return t

    def sb(self, name, shape, dtype=F32, top=False):
        return self._alloc(self.nc.sbuf_tensor, name, shape, dtype, top)

    def scr(self, name, shape, dtype=F32, nbuf=2):
        key = (name, tuple(shape), str(dtype))
        pool = self.scrpool.setdefault(key, [[], 0])
        if len(pool[0]) < nbuf:
            t = self.sb(name, shape, dtype)
            pool[0].append(t)
            return t
        t = pool[0][pool[1] % nbuf]
        pool[1] += 1
        return t

    def ps(self, name, shape, dtype=F32, top=False):
        t = self._alloc(self.nc.psum_tensor, name, shape, dtype, top)
        t.psum = True
        return t

    def _wait(self, eng, ev):
        sem, val = ev
        kid = id(sem)
        kn = self.known[eng]
        if kn.get(kid, 0) >= val:
            return
        kn[kid] = val
        self.ops[eng].append(("wait", sem, val))

    def newgen(self, t):
        t.war = _compress(t.reads + t.writes)
        t.reads = []
        t.writes = []

    def _deps(self, eng, reads, writes, pw, skip_own=None):
        def w(ev):
            if skip_own is not None and ev[0] is skip_own:
                return
            self._wait(eng, ev)
        for t in reads:
            for ev in t.writes:
                w(ev)
        for t in writes:
            if pw:
                for ev in t.war:
                    w(ev)
                for ev in t.reads:
                    w(ev)
            else:
                for ev in t.writes:
                    w(ev)
                for ev in t.reads:
                    w(ev)
                for ev in t.war:
                    w(ev)

    def _commit(self, ev, reads, writes, pw):
        for t in reads:
            if t in writes:
                continue
            t.reads.append(ev)
            if len(t.reads) > 16:
                t.reads = _compress(t.reads)
        for t in writes:
            if pw:
                t.writes.append(ev)
                if len(t.writes) > 16:
                    t.writes = _compress(t.writes)
            else:
                t.writes = [ev]
                t.reads = []
                t.war = []

    def trk(self, ap):
        return self.reg[ap.tensor.name]

    def E(self, eng, name, pw=False, **kw):
        reads, writes = [], []
        for k, v in kw.items():
            if isinstance(v, bass.AP):
                (writes if k in self.WKEYS else reads).append(self.trk(v))
        self._deps(eng, reads, writes, pw, skip_own=self.esem[eng] if eng == "tensor" else None)
        if eng != "tensor":
            for t in reads:
                if getattr(t, "psum", False):
                    for ev in t.reads:
                        if ev[0] is not self.esem[eng]:
                            self._wait(eng, ev)
        self.ecount[eng] += 1
        ev = (self.esem[eng], self.ecount[eng])
        self.ops[eng].append(("op", (lambda e, name=name, kw=kw: getattr(e, name)(**kw)), self.esem[eng]))
        self._commit(ev, reads, writes, pw)
        return ev

    def dma(self, q, out, in_, is_output=False, pw=False, **kw):
        out_t, in_t = self.trk(out), self.trk(in_)
        i = self.qn[q] % self.NQ
        self.qn[q] += 1
        sem = self.qsems[q][i]
        prev = self.quse[q][i] * 16
        if prev:
            self._wait(q, (sem, prev))
        self._deps(q, [in_t], [out_t], pw)
        self.quse[q][i] += 1
        ev = (sem, self.quse[q][i] * 16)
        self.ops[q].append(("dma", out, in_, sem, kw))
        self._commit(ev, [in_t], [out_t], pw)
        if is_output or getattr(out_t, "kind", None) == "ExternalOutput":
            self.out_events.append(ev)
            if len(self.out_events) > 64:
                self.out_events = _compress(self.out_events)
        return ev

    def collective_toplevel(self, src, dst, src_ap, dst_ap, rg):
        assert self.pstack is None
        q = "gpsimd"
        i = self.qn[q] % self.NQ
        self.qn[q] += 1
        sem = self.qsems[q][i]
        prev = self.quse[q][i] * 16
        if prev:
            self._wait(q, (sem, prev))
        self._deps(q, [src], [dst], False)
        self.quse[q][i] += 1
        ev = (sem, self.quse[q][i] * 16)
        e = self.nc.gpsimd
        for o in self.ops[q]:
            assert o[0] == "wait"
            e.wait_ge(o[1], o[2])
        self.ops[q] = []
        e.collective_compute("AllGather", ALU.bypass, replica_groups=rg, ins=[src_ap], outs=[dst_ap]).then_inc(sem, 16)
        self._commit(ev, [src], [dst], False)
        return ev

    def V(self, name, **kw):
        return self.E("vector", name, **kw)

    def A(self, name, **kw):
        return self.E("scalar", name, **kw)

    def G(self, name, **kw):
        return self.E("gpsimd", name, **kw)

    def T(self, name, **kw):
        return self.E("tensor", name, **kw)


class Builder:
    def __init__(self, mode, seg):
        self.P = Prog()
        self.mode = mode
        self.seg = seg
        self.produced = set()
        P = self.P
        self.setup_consts()

    INPUTS = {
        "xin": [NTOK, D], "cond": [2, D], "w_mod": [2, D, 6 * D], "b_mod": [2, 6 * D], "norm1_g": [2, D], "norm2_g": [2, D],
        "w_in": [2, D, IN_W], "pool_w": [2, 4, 64, 64], "pool_scale": [2, 256], "sgu_norm_g": [2, 256],
        "sgu_ws": [2, 4, 128, 128], "sgu_b": [2, 4, 128], "qa_norm_g": [2, 384], "w_uq": [2, 384, 768],
        "kva_norm_g": [2, 256], "w_ukv": [2, 256, 1024], "q_norm_g": [2, 96], "k_norm_g": [2, 96],
        "w_br_pool": [2, 256, D], "w_br_sgu": [2, 256, D], "w_br_mla": [2, 512, D], "b_gate": [2, 3 * D],
        "w_out": [2, D, D], "router_w": [D, NE], "router_b": [NE], "w_e_gate": [2, NE, D, 512],
        "w_e_up": [2, NE, D, 512], "w_e_down": [2, NE, 512, D], "ropet": [NTOK, 2, 32],
        "bands": [5, 4, 128, 128], "bandcore": [4, 4, 128, 128],
    }

    def __getattr__(self, name):
        if name in Builder.INPUTS:
            t = self.P.dram(name, Builder.INPUTS[name], F32, "ExternalInput")
            return t
        raise AttributeError(name)

    def S(self, name, shape, dtype, write):
        P = self.P
        if name in P.drams:
            return P.drams[name]
        if self.mode == "fused":
            kind = "Internal"
        else:
            kind = "ExternalOutput" if write else "ExternalInput"
        return P.dram(name, shape, dtype, kind)

    def setup_consts(self):
        P = self.P
        P.begin_phase()
        self.idf = P.sb("idf", [128, 128], F32, top=True)
        self.ident = P.sb("ident", [128, 128], BF16, top=True)
        self.ones_b = P.sb("ones_b", [128, 2], BF16, top=True)
        self.ones_f = P.sb("ones_f", [128, 128], F32, top=True)
        self.memset(self.idf, self.idf[:], 1.0, eng="gpsimd")
        P.E("gpsimd", "affine_select", out=self.idf[:], in_=self.idf[:], pattern=[[-1, 128]],
            compare_op=ALU.is_equal, fill=0.0, base=0, channel_multiplier=1)
        P.V("tensor_copy", out=self.ident[:], in_=self.idf[:])
        self.memset(self.ones_b, self.ones_b[:], 1.0)
        self.memset(self.ones_f, self.ones_f[:], 1.0)
        self.modfm = P.sb("modfm", [128, 48, 2], F32, top=True)
        self.a1 = P.sb("a1", [128, 8, 2], F32, top=True)
        self.a2 = P.sb("a2", [128, 8, 2], F32, top=True)
        self.gbc = P.sb("gbc", [128, 2, 2, D], F32, top=True)
        self.gate_all = P.sb("gate_all", [128, NT, NE], F32, top=True)
        P.end_phase()

    def _mark_write(self, t):
        pass

    def memset(self, t, ap, val, eng="vector"):
        P = self.P
        reads, writes = [], [t]
        P._deps(eng, reads, writes, False)
        P.ecount[eng] += 1
        ev = (P.esem[eng], P.ecount[eng])
        P.ops[eng].append(("op", (lambda e, ap=ap, val=val: e.memset(ap, val)), P.esem[eng]))
        P._commit(ev, reads, writes, False)

    def load_fm(self, dst_ap, src_ap, n, rows=128):
        P = self.P
        k = n // rows
        tmp = P.sb("lfm", [k, rows], F32)
        P.dma("sync", out=tmp[:], in_=src_ap.rearrange("(j p) -> j p", p=rows))
        pt = self.ps_misc
        P.T("transpose", out=pt[0:rows, 0:k], in_=tmp[:], identity=self.idf[0:k, 0:k])
        P.V("tensor_copy", out=dst_ap, in_=pt[0:rows, 0:k])

    def rstd(self, dst, src, n, cols):
        P = self.P
        P.A("activation", out=dst, in_=src, func=AF.Sqrt, scale=1.0 / n, bias=EPS)
        P.V("reciprocal", out=dst, in_=dst)

    def phase_adaln(self, l):
        P = self.P
        P.begin_phase()
        self.ps_misc = P.ps("ps_misc", [128, 512], F32)
        psm = P.ps("psm", [128, 96], F32)
        prow = [P.ps(f"prow{i}", [128, 512], F32) for i in range(4)]
        csb = P.sb("csb", [2, D], F32)
        P.dma("sync", out=csb[:], in_=self.cond.ap())
        pc = self.ps_misc
        for j in range(8):
            P.T("transpose", out=pc[:, 2 * j:2 * j + 2], in_=csb[0:2, j * 128:(j + 1) * 128], identity=self.idf[0:2, 0:2])
        scT = P.sb("scT", [128, 8, 2], BF16)
        P.A("activation", out=scT[:].rearrange("p a b -> p (a b)"), in_=pc[:, 0:16], func=AF.Silu)
        bm = P.sb("bm", [1, 6 * D], BF16)
        P.dma("gpsimd", out=bm[:], in_=self.b_mod.ap()[l:l + 1, :])
        n1 = P.sb("n1", [128, 8], F32)
        n2 = P.sb("n2", [128, 8], F32)
        self.load_fm(n1[:], self.norm1_g.ap()[l], D)
        self.load_fm(n2[:], self.norm2_g.ap()[l], D)
        wm = [P.sb(f"wm{i}", [128, 6 * D], BF16) for i in range(2)]
        gcols = [2 * D, 2 * D + 512, 5 * D, 5 * D + 512]
        macc = P.sb("modacc", [128, 96], F32)
        psms = [psm, P.ps("psm2", [128, 96], F32)]
        for c in range(8):
            w = wm[c % 2]
            pm_ = psms[c % 2]
            P.dma("gpsimd", out=w[:], in_=self.w_mod.ap()[l, c * 128:(c + 1) * 128, :])
            for j in range(48):
                P.T("matmul", out=pm_[:, 2 * j:2 * j + 2], lhsT=w[:, j * 128:(j + 1) * 128], rhs=scT[:, c, :],
                    start=True, stop=True)
            if c == 0:
                P.V("tensor_copy", out=macc[:], in_=pm_[:, 0:96])
            else:
                P.V("tensor_tensor", out=macc[:], in0=macc[:], in1=pm_[:, 0:96], op=ALU.add)
            for gi, gc in enumerate(gcols):
                P.T("matmul", out=prow[gi][0:2, :], lhsT=scT[:, c, :], rhs=w[:, gc:gc + 512], start=(c == 0), stop=False)
        pm_ = psms[0]
        for j in range(48):
            P.T("matmul", out=pm_[:, 2 * j:2 * j + 2], lhsT=bm[0:1, j * 128:(j + 1) * 128], rhs=self.ones_b[0:1, 0:2],
                start=True, stop=True)
        for gi, gc in enumerate(gcols):
            P.T("matmul", out=prow[gi][0:2, :], lhsT=self.ones_b[0:1, 0:2], rhs=bm[0:1, gc:gc + 512], start=False, stop=True)
        P.V("tensor_tensor", out=self.modfm[:].rearrange("p a b -> p (a b)"), in0=macc[:], in1=pm_[:, 0:96], op=ALU.add)
        for (a, n, off) in ((self.a1, n1, 8), (self.a2, n2, 32)):
            P.V("tensor_scalar", out=a[:], in0=self.modfm[:, off:off + 8, :], scalar1=1.0, scalar2=None, op0=ALU.add)
            P.V("tensor_tensor", out=a[:], in0=a[:], in1=n[:].unsqueeze(2).broadcast_to([128, 8, 2]), op=ALU.mult)
        grow = P.sb("grow", [2, 2048], F32)
        for gi in range(4):
            P.V("tensor_copy", out=grow[0:2, gi * 512:(gi + 1) * 512], in_=prow[gi][0:2, :])
        gd = self.P.dram(f"growd{l}_{self.seg}", [2, 2048], F32, "Internal")
        P.dma("sync", out=gd.ap(), in_=grow[:])
        for gsel in range(2):
            for cnd in range(2):
                P.dma("sync", out=self.gbc[:, gsel, cnd, :],
                      in_=gd.ap()[cnd, gsel * D:(gsel + 1) * D].partition_broadcast(128))
        P.end_phase()

    def gen_hT(self, xt, hT_ap_fn, a, shoff, cnd, gamma_scr):
        P = self.P
        sq, st, xn, pT = gamma_scr
        P.A("activation", out=sq[:], in_=xt[:], func=AF.Square, accum_out=st[:, 0:1])
        yield
        P.A("activation", out=st[:, 1:2], in_=st[:, 0:1], func=AF.Sqrt, scale=1.0 / D, bias=EPS)
        yield
        P.V("reciprocal", out=st[:, 1:2], in_=st[:, 1:2])
        yield
        P.V("tensor_scalar", out=xn[:], in0=xt[:], scalar1=st[:, 1:2], scalar2=None, op0=ALU.mult)
        yield
        for c in range(8):
            P.T("transpose", out=pT[:, c, :], in_=xn[:, c * 128:(c + 1) * 128], identity=self.ident[:])
        for c in range(8):
            P.A("activation", out=hT_ap_fn(c), in_=pT[:, c, :], func=AF.Identity,
                scale=a[:, c, cnd:cnd + 1], bias=self.modfm[:, shoff + c, cnd:cnd + 1], pw=True)
        yield

    def emit_hT(self, xt, hT_ap_fn, a, shoff, cnd, gamma_scr):
        for _ in self.gen_hT(xt, hT_ap_fn, a, shoff, cnd, gamma_scr):
            pass

    @staticmethod
    def run_chains(chains):
        chains = list(chains)
        while chains:
            nxt = []
            for g in chains:
                try:
                    next(g)
                    nxt.append(g)
                except StopIteration:
                    pass
            chains = nxt

    def blocks(self, l, with_ctx, size=4):
        bl = [list(range(i, i + size)) for i in range(0, 32, size)]
        if with_ctx:
            bl.append([32, 33])
        return bl

    def xsrc(self, l):
        if l == 0:
            return self.xin
        return self.S("xres", [NTOK, D], F32, write=False)

    def phase_p1(self, l):
        P = self.P
        P.begin_phase()
        xsrc = self.xsrc(l)
        qx = self.S(f"qx{l}", [96, 8, NTOK], BF16, True)
        kxo = [self.S(f"kx_own{l}_{c}", [2, 96, NTOK], BF16, True) for c in range(4)]
        vxo = [self.S(f"vx_own{l}_{c}", [2, 128, NT, 65], BF16, True) for c in range(4)]
        pall_d = self.S(f"pall{l}", [128, NT, 256], BF16, True)
        pe = self.S(f"pe_own{l}", [16, 256], BF16, True)
        pT = [P.ps("pT0", [128, 8, 128], BF16)] * 2
        pfm = [P.ps("pfm0", [128, 512], F32)] * 2
        ptm = P.ps("ptm", [128, 1024], F32)
        ptm2 = P.ps("ptm2", [128, 1024], F32)
        psmall = P.ps("psmall", [128, 512], F32)
        self.ps_misc = psmall
        ptr = P.ps("ptr", [128, 8, 128], BF16)
        ptr2 = ptr
        w1 = P.sb("w1", [128, 8, 672], BF16)
        wp = P.sb("wp", [128, 8, 256], BF16)
        win = self.w_in.ap()[l].rearrange("(c p) n -> p c n", p=128)
        P.dma("gpsimd", out=w1[:], in_=win[:, :, OFF_QA:OFF_GATE])
        P.dma("gpsimd", out=wp[:], in_=win[:, :, 0:256])
        wuq = P.sb("wuq", [128, 3, 768], BF16)
        P.dma("gpsimd", out=wuq[:], in_=self.w_uq.ap()[l].rearrange("(c p) n -> p c n", p=128))
        wukv = P.sb("wukv", [128, 2, 1024], BF16)
        P.dma("gpsimd", out=wukv[:], in_=self.w_ukv.ap()[l].rearrange("(c p) n -> p c n", p=128))
        gfm = P.sb("gfm", [128, 5], F32)
        self.load_fm(gfm[:, 0:3], self.qa_norm_g.ap()[l], 384)
        self.load_fm(gfm[:, 3:5], self.kva_norm_g.ap()[l], 256)
        qg = P.sb("qg", [128, 96], F32)
        kg = P.sb("kg", [128, 96], F32)
        P.dma("sync", out=qg[:], in_=self.q_norm_g.ap()[l].partition_broadcast(128))
        P.dma("sync", out=kg[:], in_=self.k_norm_g.ap()[l].partition_broadcast(128))
        xts = [P.sb(f"xt{i}", [128, D], F32) for i in range(2)]
        sq = P.sb("sq", [128, D], BF16)
        xn = P.sb("xn", [128, D], BF16)
        hTs = [P.sb(f"hT{i}", [128, 8, 512], BF16) for i in range(2)]
        sq_b = P.sb("sq_b", [128, D], BF16)
        xn_b = P.sb("xn_b", [128, D], BF16)
        sqT = P.sb("sqT", [128, 5, 512], BF16)
        aT = P.sb("aT", [128, 5, 512], BF16)
        p_all = P.sb("p_all", [128, NT, 256], BF16)
        Qblk = [P.sb("Qblk0", [96, 8, 512], BF16)]
        Kblk = [P.sb("Kblk0", [96, 8, 512], BF16)]
        Vblk = [P.sb(f"Vblk{i}", [128, 8, 4, 65], BF16) for i in range(2)]
        for v in Vblk:
            self.memset(v, v[:], 1.0)
        rt = [P.sb(f"rt{i}", [128, 2, 32], F32) for i in range(4)]
        ti = 0
        stop = getattr(self, "stop", 99)
        blks_all = self.blocks(l, True)

        def hT_gens(bi):
            blk = blks_all[bi]
            hT = hTs[bi % 2]
            P.newgen(hT)

            def gen(which):
                scr_ = (sq, xn) if which == 0 else (sq_b, xn_b)
                xt = xts[which]
                for j, tile in enumerate(blk):
                    if j % 2 != which:
                        continue
                    cnd = 1 if tile >= 32 else 0
                    P.dma("sync", out=xt[:], in_=xsrc.ap()[tile * 128:(tile + 1) * 128, :])
                    st = P.scr("st", [128, 2], F32, nbuf=4)
                    yield from self.gen_hT(xt, lambda c, hT=hT, j=j: hT[:, c, j * 128:(j + 1) * 128], self.a1, 0, cnd,
                                           (scr_[0], st, scr_[1], pT[0]))
            return [gen(0), gen(1)]

        if stop > 1:
            self.run_chains(hT_gens(0))
        for bi, blk in enumerate(blks_all if stop > 1 else []):
            nt = len(blk)
            N = nt * 128
            hT = hTs[bi % 2]
            if stop <= 2:
                continue
            P.newgen(sqT)
            P.newgen(aT)
            for ci in range(5):
                pf = pfm[ci % 2]
                for c in range(8):
                    P.T("matmul", out=pf[:, 0:N], lhsT=w1[:, c, ci * 128:(ci + 1) * 128], rhs=hT[:, c, 0:N],
                        start=(c == 0), stop=(c == 7))
                P.A("activation", out=sqT[:, ci, 0:N], in_=pf[:, 0:N], func=AF.Square, pw=True)
                P.V("tensor_scalar", out=aT[:, ci, 0:N], in0=pf[:, 0:N], scalar1=gfm[:, ci:ci + 1], scalar2=None,
                    op0=ALU.mult, pw=True)
            if stop <= 3:
                continue
            Qb_, Kb_, Vb_ = Qblk[0], Kblk[0], Vblk[bi % 2]
            P.newgen(Qb_)
            P.newgen(Kb_)
            t0 = blk[0]
            chains = []
            for j, tile in enumerate(blk):
                js = slice(j * 128, (j + 1) * 128)
                rtt = rt[j % 4]
                P.dma("sync", out=rtt[:], in_=self.ropet.ap()[tile * 128:(tile + 1) * 128])
                for ci in range(3):
                    P.T("matmul", out=psmall[:, 0:1], lhsT=sqT[:, ci, js], rhs=self.ones_b[:, 0:1], start=(ci == 0), stop=(ci == 2))
                for ci in range(2):
                    P.T("matmul", out=psmall[:, 1:2], lhsT=sqT[:, 3 + ci, js], rhs=self.ones_b[:, 0:1], start=(ci == 0), stop=(ci == 1))
                for c in range(8):
                    P.T("matmul", out=psmall[:, 8:40], lhsT=hT[:, c, js], rhs=w1[:, c, 640:672], start=(c == 0), stop=(c == 7))
                for c in range(8):
                    P.T("matmul", out=psmall[:, 64:320], lhsT=hT[:, c, js], rhs=wp[:, c, :], start=(c == 0), stop=(c == 7))
                st = P.scr("stq", [128, 8], F32, nbuf=4)
                junk = P.scr("junk", [128, 32], F32, nbuf=4)
                kr = P.scr("kr", [128, 32], F32, nbuf=4)
                P.A("activation", out=st[:, 0:1], in_=psmall[:, 0:1], func=AF.Sqrt, scale=1.0 / 384, bias=EPS)
                P.A("activation", out=st[:, 1:2], in_=psmall[:, 1:2], func=AF.Sqrt, scale=1.0 / 256, bias=EPS)
                P.A("activation", out=junk[:], in_=psmall[:, 8:40], func=AF.Square, accum_out=st[:, 2:3])
                P.A("activation", out=st[:, 3:4], in_=st[:, 2:3], func=AF.Sqrt, scale=1.0 / 32, bias=EPS)
                P.V("reciprocal", out=st[:, 4:8], in_=st[:, 0:4])
                rq, rkv, rkr = st[:, 4:5], st[:, 5:6], st[:, 7:8]
                P.A("activation", out=p_all[:, tile, :], in_=psmall[:, 64:320], func=AF.Identity, pw=True)
                P.A("activation", out=kr[:], in_=psmall[:, 8:40], func=AF.Identity, scale=rkr)
                need_q = not (l == 1 and tile >= 32)
                qs = P.scr("qs", [128, 8, 96], F32, nbuf=4)
                if need_q:
                    for ci in range(3):
                        P.T("matmul", out=ptm[:, 0:512], lhsT=aT[:, ci, js], rhs=wuq[:, ci, 0:512], start=(ci == 0), stop=(ci == 2))
                    for ci in range(3):
                        P.T("matmul", out=ptm[:, 512:768], lhsT=aT[:, ci, js], rhs=wuq[:, ci, 512:768], start=(ci == 0), stop=(ci == 2))
                    P.A("activation", out=qs[:].rearrange("p a b -> p (a b)"), in_=ptm[:, 0:768], func=AF.Identity, scale=rq)
                ptk = ptm2
                for half in range(2):
                    for ci in range(2):
                        P.T("matmul", out=ptk[:, half * 512:(half + 1) * 512], lhsT=aT[:, 3 + ci, js],
                            rhs=wukv[:, ci, half * 512:(half + 1) * 512], start=(ci == 0), stop=(ci == 1))
                ks = P.scr("ks", [128, 8, 128], F32, nbuf=4)
                P.A("activation", out=ks[:].rearrange("p a b -> p (a b)"), in_=ptk[:, 0:1024], func=AF.Identity, scale=rkv)

                def chain_q(j=j, js=js, qs=qs, rtt=rtt):
                    sq2 = P.scr("sq2", [128, 8, 96], F32, nbuf=4)
                    st2 = P.scr("st2", [128, 16], F32, nbuf=4)
                    P.G("tensor_tensor", out=sq2[:], in0=qs[:], in1=qs[:], op=ALU.mult); yield
                    P.V("tensor_reduce", out=st2[:, 0:8], in_=sq2[:, :, 0:64], axis=AX.X, op=ALU.add); yield
                    P.V("tensor_reduce", out=st2[:, 8:16], in_=sq2[:, :, 64:96], axis=AX.X, op=ALU.add); yield
                    P.A("activation", out=st2[:, 0:8], in_=st2[:, 0:8], func=AF.Sqrt, scale=1.0 / 64, bias=EPS); yield
                    P.A("activation", out=st2[:, 8:16], in_=st2[:, 8:16], func=AF.Sqrt, scale=1.0 / 32, bias=EPS); yield
                    P.V("reciprocal", out=st2[:], in_=st2[:]); yield
                    P.V("tensor_tensor", out=qs[:, :, 0:64], in0=qs[:, :, 0:64],
                        in1=st2[:, 0:8].unsqueeze(2).broadcast_to([128, 8, 64]), op=ALU.mult); yield
                    P.V("tensor_tensor", out=qs[:, :, 64:96], in0=qs[:, :, 64:96],
                        in1=st2[:, 8:16].unsqueeze(2).broadcast_to([128, 8, 32]), op=ALU.mult); yield
                    P.V("tensor_tensor", out=qs[:], in0=qs[:], in1=qg[:].unsqueeze(1).broadcast_to([128, 8, 96]), op=ALU.mult); yield
                    Qb = P.scr("Qb", [128, 8, 96], BF16, nbuf=4)
                    xs = P.scr("xs", [128, 8, 32], F32, nbuf=4)
                    r5 = qs[:, :, 64:96].rearrange("p h (a b k) -> p h a b k", a=2, b=2)
                    x5 = xs[:].rearrange("p h (a b k) -> p h a b k", a=2, b=2)
                    P.G("tensor_copy", out=x5[:, :, :, 0, :], in_=r5[:, :, :, 1, :]); yield
                    P.G("tensor_copy", out=x5[:, :, :, 1, :], in_=r5[:, :, :, 0, :]); yield
                    P.G("tensor_tensor", out=xs[:], in0=xs[:], in1=rtt[:, 1:2, :].broadcast_to([128, 8, 32]), op=ALU.mult); yield
                    P.V("tensor_tensor", out=sq2[:, :, 0:32], in0=qs[:, :, 64:96], in1=rtt[:, 0:1, :].broadcast_to([128, 8, 32]), op=ALU.mult); yield
                    P.V("tensor_tensor", out=Qb[:, :, 64:96], in0=sq2[:, :, 0:32], in1=xs[:], op=ALU.add); yield
                    P.A("activation", out=Qb[:, :, 0:64], in_=qs[:, :, 0:64], func=AF.Identity); yield
                    for h in range(8):
                        P.T("transpose", out=ptr[0:96, h, :], in_=Qb[:, h, :], identity=self.ident[:])
                    P.V("tensor_copy", out=Qb_[:, :, js], in_=ptr[0:96, :, :], pw=True); yield

                def chain_kv(j=j, js=js, ks=ks, kr=kr, rtt=rtt):
                    P.G("tensor_copy", out=Vb_[:, :, j, 0:64], in_=ks[:, :, 64:128]); yield
                    sq3 = P.scr("sq3", [128, 8, 64], F32, nbuf=4)
                    st3 = P.scr("st3", [128, 8], F32, nbuf=4)
                    P.G("tensor_tensor", out=sq3[:], in0=ks[:, :, 0:64], in1=ks[:, :, 0:64], op=ALU.mult); yield
                    P.V("tensor_reduce", out=st3[:], in_=sq3[:], axis=AX.X, op=ALU.add); yield
                    P.A("activation", out=st3[:], in_=st3[:], func=AF.Sqrt, scale=1.0 / 64, bias=EPS); yield
                    P.V("reciprocal", out=st3[:], in_=st3[:]); yield
                    Kb = P.scr("Kb", [128, 8, 96], BF16, nbuf=4)
                    P.V("tensor_tensor", out=sq3[:], in0=ks[:, :, 0:64], in1=st3[:].unsqueeze(2).broadcast_to([128, 8, 64]), op=ALU.mult); yield
                    P.V("tensor_tensor", out=Kb[:, :, 0:64], in0=sq3[:], in1=kg[:, 0:64].unsqueeze(1).broadcast_to([128, 8, 64]), op=ALU.mult); yield
                    kxs = P.scr("kxs", [128, 32], F32, nbuf=4)
                    kr2 = P.scr("kr2", [128, 32], F32, nbuf=4)
                    P.V("tensor_tensor", out=kr[:], in0=kr[:], in1=kg[:, 64:96], op=ALU.mult); yield
                    k4 = kr[:].rearrange("p (a b k) -> p a b k", a=2, b=2)
                    kx4 = kxs[:].rearrange("p (a b k) -> p a b k", a=2, b=2)
                    P.G("tensor_copy", out=kx4[:, :, 0, :], in_=k4[:, :, 1, :]); yield
                    P.G("tensor_copy", out=kx4[:, :, 1, :], in_=k4[:, :, 0, :]); yield
                    P.G("tensor_tensor", out=kxs[:], in0=kxs[:], in1=rtt[:, 1, :], op=ALU.mult); yield
                    P.V("tensor_tensor", out=kr2[:], in0=kr[:], in1=rtt[:, 0, :], op=ALU.mult); yield
                    P.V("tensor_tensor", out=kr2[:], in0=kr2[:], in1=kxs[:], op=ALU.add); yield
                    P.V("tensor_copy", out=Kb[:, :, 64:96], in_=kr2[:].unsqueeze(1).broadcast_to([128, 8, 32])); yield
                    for h in range(8):
                        P.T("transpose", out=ptr2[0:96, h, :], in_=Kb[:, h, :], identity=self.ident[:])
                    P.A("activation", out=Kb_[:, :, js], in_=ptr2[0:96, :, :], func=AF.Identity, pw=True); yield

                if need_q:
                    chains.append(chain_q())
                chains.append(chain_kv())
            if bi + 1 < len(blks_all):
                chains += hT_gens(bi + 1)
            self.run_chains(chains)
            if stop <= 6:
                continue
            if not (l == 1 and t0 >= 32):
                P.dma("sync", out=qx.ap()[:, :, t0 * 128:t0 * 128 + N], in_=Qb_[:, :, 0:N])
            for c in range(4):
                P.dma("sync", out=kxo[c].ap()[:, :, t0 * 128:t0 * 128 + N].rearrange("h d t -> d h t"), in_=Kb_[:, 2 * c:2 * c + 2, 0:N])
                P.dma("sync", out=vxo[c].ap()[:, :, t0:t0 + nt, :].rearrange("h p t e -> p h t e"), in_=Vb_[:, 2 * c:2 * c + 2, 0:nt, :])
        P.dma("sync", out=pall_d.ap(), in_=p_all[:])
        P.dma("sync", out=pe.ap()[0:8, :], in_=p_all[0:8, 0, :])
        P.dma("sync", out=pe.ap()[8:16, :], in_=p_all[120:128, 31, :])
        P.end_phase()

    def phase_exchange(self, l, own_phase=True):
        P = self.P
        if own_phase:
            P.begin_phase()
        rg = [[0, 1], [2, 3], [4, 5], [6, 7]]
        q = "gpsimd"
        names = []
        for c in range(4):
            names += [f"kx_own{l}_{c}", f"vx_own{l}_{c}"]
        names.append(f"pe_own{l}")
        for nm in names:
            src = P.drams[nm]
            shp = list(src.ap().shape)
            dst = P.dram(nm.replace("_own", "_all"), [2] + shp, BF16, "Internal")
            sap, dap = src.ap(), dst.ap()
            if len(shp) == 3:
                sap = sap.rearrange("a b c -> (a b) c")
                dap = dap.rearrange("r a b c -> (r a b) c")
            elif len(shp) == 4:
                sap = sap.rearrange("a b c d -> (a b) (c d)")
                dap = dap.rearrange("r a b c d -> (r a b) (c d)")
            else:
                dap = dap.rearrange("r a b -> (r a) b")
            if P.cccount:
                P._wait(q, (P.ccsem, P.cccount))
            P._deps(q, [src], [dst], False)
            P.cccount += 1
            ev = (P.ccsem, P.cccount)
            P.ops[q].append(("cc", sap.opt(), dap.opt(), P.ccsem, rg))
            P._commit(ev, [src], [dst], False)
        if own_phase:
            P.end_phase()

    def phase_attn(self, l, with_ctx):
        P = self.P
        self.mixw = None
        if self.mode == "fused":
            P.span_begin()
            self.mixw = dict(
                wuv=P.sb_span("wuv", [128, 8, 512], BF16), wg=P.sb_span("wg", [128, 8, 3 * D], BF16),
                wbp=P.sb_span("wbp", [64, 4, D], BF16), wbs=P.sb_span("wbs", [64, 4, D], BF16),
                wbm=P.sb_span("wbm", [64, 8, D], BF16))
        P.begin_phase()
        if self.mode == "fused":
            self.phase_exchange(l, own_phase=False)
            self.load_mix_weights(l, self.mixw)
        qx = self.S(f"qx{l}", [96, 8, NTOK], BF16, False)
        kxas = [self.S(f"kx_all{l}_{c}", [2, 2, 96, NTOK], BF16, False) for c in range(4)]
        vxas = [self.S(f"vx_all{l}_{c}", [2, 2, 128, NT, 65], BF16, False) for c in range(4)]
        yatt = self.S(f"yatt{l}", [64, 8, NTOK], BF16, True)
        NKT = 66
        Kh = [P.sb(f"Kh{i}", [96, NKT * 128], BF16) for i in range(2)]
        Vh = [P.sb(f"Vh{i}", [128, NKT, 65], BF16) for i in range(2)]
        Qh = [P.sb(f"Qh{i}", [96, NTOK], BF16) for i in range(2)]
        NPS = 3
        pS = [P.ps(f"pS{i}", [128, 2, 512], F32) for i in range(NPS)]
        pacc = [P.ps(f"pacc{i}", [128, 512], F32) for i in range(2)]
        NPB = 3
        Pb = [P.sb(f"Pb{i}", [128, 2, 512], BF16) for i in range(NPB)]
        osb = [P.sb(f"osb{i}", [65, 512], F32) for i in range(2)]
        rc = [P.sb(f"rc{i}", [65, 512], F32) for i in range(2)]
        rbc = [P.sb(f"rbc{i}", [64, 512], F32) for i in range(2)]
        yb = [P.sb(f"yb{i}", [64, 512], BF16) for i in range(2)]
        rden = [P.dram(f"rden{l}_{i}", [1, 512], F32, "Internal") for i in range(2)]
        LOOK = 2
        iters = []
        heads_first_iter = {}
        qslot = 0
        for h in range(8):
            qblocks = [(i * 512, 512, list(range(NKT))) for i in range(8)]
            if with_ctx:
                qblocks.append((NLAT, 256, [32, 33]))
            heads_first_iter[h] = len(iters)
            for (q0, qn, kts) in qblocks:
                assert len(kts) % 2 == 0
                for ki in range(0, len(kts), 2):
                    assert kts[ki + 1] == kts[ki] + 1
                    iters.append((h, qslot, q0, qn, kts[ki], ki == 0, ki == len(kts) - 2))
                qslot += 1
        loaded = set()

        def load_head(h):
            if h in loaded or h >= 8:
                return
            loaded.add(h)
            K_, V_, Q_ = Kh[h % 2], Vh[h % 2], Qh[h % 2]
            kxa, vxa, hh = kxas[h // 2], vxas[h // 2], h % 2
            P.dma("sync", out=K_[:, 0:NTOK], in_=kxa.ap()[0, hh, :, :])
            P.dma("sync", out=K_[:, NTOK:NTOK + NLAT], in_=kxa.ap()[1, hh, :, 0:NLAT])
            P.dma("sync", out=V_[:, 0:NT, :], in_=vxa.ap()[0, hh, :, :, :])
            P.dma("sync", out=V_[:, NT:NKT, :], in_=vxa.ap()[1, hh, :, 0:32, :])
            P.dma("sync", out=Q_[:], in_=qx.ap()[:, h, :])

        def emit_S(i):
            h, qs, q0, qn, kt, first, last = iters[i]
            load_head(h)
            for half in range(2):
                P.T("matmul", out=pS[i % NPS][:, half, 0:qn], lhsT=Kh[h % 2][:, (kt + half) * 128:(kt + half + 1) * 128],
                    rhs=Qh[h % 2][:, q0:q0 + qn], start=True, stop=True)

        pending = []

        def make_epilogue(h, qs, q0, qn):
            acc = pacc[qs % 2]
            o_, r_, y_ = osb[qs % 2], rc[qs % 2], yb[qs % 2]

            def part1():
                P.V("tensor_copy", out=o_[:, 0:qn], in_=acc[0:65, 0:qn])
                P.V("reciprocal", out=r_[64:65, 0:qn], in_=o_[64:65, 0:qn])

            def part2():
                rd, rb_ = rden[qs % 2], rbc[qs % 2]
                P.dma("sync", out=rd.ap()[:, 0:qn], in_=r_[64:65, 0:qn])
                P.dma("sync", out=rb_[:, 0:qn], in_=rd.ap()[0, 0:qn].partition_broadcast(64))
                P.V("tensor_tensor", out=y_[:, 0:qn], in0=o_[0:64, 0:qn], in1=rb_[:, 0:qn], op=ALU.mult)
                P.dma("sync", out=yatt.ap()[:, h, q0:q0 + qn], in_=y_[:, 0:qn])
            return part1, part2

        n_it = len(iters)
        for i in range(min(LOOK, n_it)):
            emit_S(i)
        for i in range(n_it):
            h, qs, q0, qn, kt, first, last = iters[i]
            P.A("activation", out=Pb[i % NPB][:, :, 0:qn], in_=pS[i % NPS][:, :, 0:qn], func=AF.Exp, scale=SM_SCALE)
            if i + LOOK < n_it:
                emit_S(i + LOOK)
            for half in range(2):
                P.T("matmul", out=pacc[qs % 2][0:65, 0:qn], lhsT=Vh[h % 2][:, kt + half, :], rhs=Pb[i % NPB][:, half, 0:qn],
                    start=(first and half == 0), stop=(last and half == 1))
            if last:
                p1, p2 = make_epilogue(h, qs, q0, qn)
                p1()
                pending.append((i + 1, p2))
            if i == heads_first_iter[h] + 4:
                load_head(h + 1)
            while pending and pending[0][0] <= i:
                pending.pop(0)[1]()
        for _, fn in pending:
            fn()
        P.end_phase()

    def load_mix_weights(self, l, w):
        P = self.P
        win = self.w_in.ap()[l].rearrange("(c p) n -> p c n", p=128)
        P.dma("gpsimd", out=w["wuv"][:], in_=win[:, :, OFF_U:OFF_QA])
        P.newgen(w["wg"])
        for c in range(8):
            P.dma("gpsimd", out=w["wg"][:, c, :], in_=win[:, c, OFF_GATE:IN_W], pw=True)
        P.dma("gpsimd", out=w["wbp"][:], in_=self.w_br_pool.ap()[l].rearrange("(g c) n -> c g n", c=64))
        P.dma("gpsimd", out=w["wbs"][:], in_=self.w_br_sgu.ap()[l].rearrange("(g c) n -> c g n", c=64))
        P.dma("gpsimd", out=w["wbm"][:], in_=self.w_br_mla.ap()[l].rearrange("(g c) n -> c g n", c=64))

    def phase_mix(self, l, with_ctx):
        P = self.P
        P.begin_phase()
        xsrc = self.xsrc(l)
        pall_d = self.S(f"pall{l}", [128, NT, 256], BF16, False)
        pea = self.S(f"pe_all{l}", [2, 16, 256], BF16, False)
        yatt = self.S(f"yatt{l}", [64, 8, NTOK], BF16, False)
        mTd = self.S(f"mT{l}", [128, 8, NTOK], BF16, True)
        pT = P.ps("pT", [128, 8, 128], BF16)
        pA = [P.ps(f"pA{i}", [128, 512], F32) for i in range(5)]
        pS = P.ps("pSm", [128, 512], F32)
        pD = P.ps("pD", [128, 512], F32)
        self.ps_misc = pS
        if getattr(self, "mixw", None) is not None:
            w = self.mixw
        else:
            w = dict(wuv=P.sb("wuv", [128, 8, 512], BF16), wg=P.sb("wg", [128, 8, 3 * D], BF16),
                     wbp=P.sb("wbp", [64, 4, D], BF16), wbs=P.sb("wbs", [64, 4, D], BF16), wbm=P.sb("wbm", [64, 8, D], BF16))
            self.load_mix_weights(l, w)
        wuv, wg, wbp, wbs, wbm = w["wuv"], w["wg"], w["wbp"], w["wbs"], w["wbm"]
        pw_ = P.sb("poolw", [64, 4, 64], BF16)
        P.dma("gpsimd", out=pw_[:], in_=self.pool_w.ap()[l].rearrange("g c d -> c g d"))
        bnd = P.sb("bnd", [128, 5, 4, 128], BF16)
        bnc = P.sb("bnc", [128, 4, 4, 128], BF16)
        P.dma("gpsimd", out=bnd[:], in_=self.bands.ap().rearrange("n g s t -> s n g t"))
        P.dma("gpsimd", out=bnc[:], in_=self.bandcore.ap().rearrange("n g s t -> s n g t"))
        sng = P.sb("sng", [128, 256], F32)
        P.dma("sync", out=sng[:], in_=self.sgu_norm_g.ap()[l].partition_broadcast(128))
        sbb = P.sb("sbb", [64, 4, 128], F32)
        P.newgen(sbb)
        for g in range(4):
            P.dma("sync", out=sbb[:, g, :], in_=self.sgu_b.ap()[l, g].partition_broadcast(64), pw=True)
        psc = P.sb("psc", [64, 4], F32)
        self.load_fm(psc[:], self.pool_scale.ap()[l], 256, rows=64)
        bg = P.sb("bg", [128, 24], F32)
        self.load_fm(bg[:], self.b_gate.ap()[l], 3 * D)
        wsn = P.sb("wsn", [128, 4, 128], BF16)
        P.dma("gpsimd", out=wsn[:], in_=self.sgu_ws.ap()[l].rearrange("g i j -> i g j"))
        wsT = P.sb("wsT", [128, 4, 128], BF16)
        for g in range(4):
            P.T("transpose", out=pT[:, g, :], in_=wsn[:, g, :], identity=self.ident[:])
        P.V("tensor_copy", out=wsT[:], in_=pT[:, 0:4, :])
        p_all = P.sb("p_all", [128, NT, 256], BF16)
        P.dma("sync", out=p_all[:], in_=pall_d.ap())
        pedge = P.sb("pedge", [32, 256], BF16)
        P.dma("sync", out=pedge[:], in_=pea.ap().rearrange("r a b -> (r a) b"))
        xts = [P.sb(f"xt{i}", [128, D], F32) for i in range(2)]
        sq = P.sb("sq", [128, D], BF16)
        xn = P.sb("xn", [128, D], BF16)
        hTs = [P.sb(f"hT{i}", [128, 8, 256], BF16) for i in range(2)]
        dT = P.sb("dT", [64, 4, 256], BF16)
        ypT = P.sb("ypT", [64, 4, 256], BF16)
        ysT = P.sb("ysT", [64, 4, 256], BF16)
        yaT = P.sb("yaT", [64, 8, 256], BF16)
        vn = P.sb("vn", [128, 2, 256], BF16)
        mT = P.sb("mT", [128, 8, 256], BF16)
        ti = [0]
        blks = self.blocks(l, with_ctx, 2)

        def produce_hT(bi):
            blk = blks[bi]
            hT = hTs[bi % 2]
            cnd = 1 if blk[0] >= 32 else 0
            P.newgen(hT)
            for j, tile in enumerate(blk):
                xt = xts[ti[0] % 2]
                ti[0] += 1
                P.dma("sync", out=xt[:], in_=xsrc.ap()[tile * 128:(tile + 1) * 128, :])
                st = P.scr("st", [128, 2], F32)
                self.emit_hT(xt, lambda c, j=j, hT=hT: hT[:, c, j * 128:(j + 1) * 128], self.a1, 0, cnd, (sq, st, xn, pT))

        produce_hT(0)
        for bi, blk in enumerate(blks):
            nt = len(blk)
            N = nt * 128
            t0 = blk[0]
            cnd = 1 if t0 >= 32 else 0
            hT = hTs[bi % 2]
            P.dma("sync", out=yaT[:, :, 0:N], in_=yatt.ap()[:, :, t0 * 128:t0 * 128 + N])
            P.newgen(vn)
            for j in range(nt):
                js = slice(j * 128, (j + 1) * 128)
                for c in range(8):
                    P.T("matmul", out=pS[:, 0:256], lhsT=hT[:, c, js], rhs=wuv[:, c, 256:512], start=(c == 0), stop=(c == 7))
                gv = P.scr("gv", [128, 256], F32)
                jk = P.scr("jk", [128, 256], F32)
                sv = P.scr("sv", [128, 2], F32)
                P.A("activation", out=gv[:], in_=pS[:, 0:256], func=AF.Gelu_apprx_tanh)
                P.A("activation", out=jk[:], in_=gv[:], func=AF.Square, accum_out=sv[:, 0:1])
                self.rstd(sv[:, 1:2], sv[:, 0:1], 256, 1)
                P.V("scalar_tensor_tensor", out=vn[:, j, :], in0=gv[:], scalar=sv[:, 1:2], in1=sng[:], op0=ALU.mult, op1=ALU.mult, pw=True)
            P.newgen(dT)
            for g in range(4):
                gs = slice(g * 64, (g + 1) * 64)
                for j, tile in enumerate(blk):
                    js = slice(j * 128, (j + 1) * 128)
                    if tile == 0:
                        srcs = [(pedge[0:32, gs], bnc[0:32, 2, g, :]), (p_all[:, 0, gs], bnc[:, 0, g, :]), (p_all[:, 1, gs], bnd[:, 2, g, :])]
                    elif tile == 31:
                        srcs = [(p_all[:, 30, gs], bnd[:, 1, g, :]), (p_all[:, 31, gs], bnc[:, 1, g, :]), (pedge[0:32, gs], bnc[0:32, 3, g, :])]
                    elif tile == 32:
                        srcs = [(p_all[:, 32, gs], bnd[:, 3, g, :]), (p_all[:, 33, gs], bnd[:, 2, g, :])]
                    elif tile == 33:
                        srcs = [(p_all[:, 32, gs], bnd[:, 1, g, :]), (p_all[:, 33, gs], bnd[:, 4, g, :])]
                    else:
                        srcs = [(p_all[:, tile - 1, gs], bnd[:, 1, g, :]), (p_all[:, tile, gs], bnd[:, 0, g, :]), (p_all[:, tile + 1, gs], bnd[:, 2, g, :])]
                    for si, (lh, rh) in enumerate(srcs):
                        P.T("matmul", out=pD[0:64, js], lhsT=lh, rhs=rh, start=(si == 0), stop=(si == len(srcs) - 1))
                P.V("tensor_copy", out=dT[:, g, 0:N], in_=pD[0:64, 0:N], pw=True)
            P.newgen(ypT)
            for g in range(4):
                P.T("matmul", out=pD[0:64, 0:N], lhsT=pw_[:, g, :], rhs=dT[:, g, 0:N], start=True, stop=True)
                P.V("tensor_scalar", out=ypT[:, g, 0:N], in0=pD[0:64, 0:N], scalar1=psc[:, g:g + 1], scalar2=None, op0=ALU.mult, pw=True)
            P.newgen(ysT)
            for g in range(4):
                gs = slice(g * 64, (g + 1) * 64)
                pa = pA[g % 2]
                for c in range(8):
                    P.T("matmul", out=pa[0:64, 0:N], lhsT=wuv[:, c, g * 64:(g + 1) * 64], rhs=hT[:, c, 0:N], start=(c == 0), stop=(c == 7))
                ug = P.scr("ug", [64, 256], F32)
                P.A("activation", out=ug[:, 0:N], in_=pa[0:64, 0:N], func=AF.Gelu_apprx_tanh)
                for j in range(nt):
                    P.T("matmul", out=pD[0:64, j * 128:(j + 1) * 128], lhsT=vn[:, j, gs], rhs=wsT[:, g, :], start=True, stop=True)
                mx = P.scr("mx", [64, 2, 128], F32)
                P.V("tensor_tensor", out=mx[:, 0:nt, :], in0=pD[0:64, 0:N].rearrange("p (a b) -> p a b", b=128),
                    in1=sbb[:, g:g + 1, :].broadcast_to([64, nt, 128]), op=ALU.add)
                P.V("tensor_tensor", out=ysT[:, g, 0:N], in0=mx[:, 0:nt, :].rearrange("p a b -> p (a b)"), in1=ug[:, 0:N], op=ALU.mult, pw=True)
            if bi + 1 < len(blks):
                produce_hT(bi + 1)
            P.newgen(mT)
            for dc in range(8):
                ds_ = slice(dc * 128, (dc + 1) * 128)
                gts = []
                for bi3 in range(3):
                    gc = bi3 * 8 + dc
                    pa = pA[bi3]
                    for c in range(8):
                        P.T("matmul", out=pa[:, 0:N], lhsT=wg[:, c, gc * 128:(gc + 1) * 128], rhs=hT[:, c, 0:N], start=(c == 0), stop=(c == 7))
                    gt = P.scr(f"gt{bi3}", [128, 256], BF16)
                    P.A("activation", out=gt[:, 0:N], in_=pa[:, 0:N], func=AF.Sigmoid, bias=bg[:, gc:gc + 1])
                    gts.append(gt)
                macc = P.scr("macc", [128, 256], F32, nbuf=1)
                tmpb = P.scr("tmpb", [128, 256], F32, nbuf=1)
                tmpc = P.scr("tmpc", [128, 256], F32, nbuf=1)
                pa = pA[3]
                for g in range(4):
                    P.T("matmul", out=pa[:, 0:N], lhsT=wbp[:, g, ds_], rhs=ypT[:, g, 0:N], start=(g == 0), stop=(g == 3))
                P.V("tensor_tensor", out=macc[:, 0:N], in0=pa[:, 0:N], in1=gts[0][:, 0:N], op=ALU.mult)
                pa = pA[4]
                for g in range(4):
                    P.T("matmul", out=pa[:, 0:N], lhsT=wbs[:, g, ds_], rhs=ysT[:, g, 0:N], start=(g == 0), stop=(g == 3))
                P.V("tensor_tensor", out=tmpb[:, 0:N], in0=pa[:, 0:N], in1=gts[1][:, 0:N], op=ALU.mult)
                P.G("tensor_tensor", out=macc[:, 0:N], in0=macc[:, 0:N], in1=tmpb[:, 0:N], op=ALU.add)
                pa = pA[3]
                for hh in range(8):
                    P.T("matmul", out=pa[:, 0:N], lhsT=wbm[:, hh, ds_], rhs=yaT[:, hh, 0:N], start=(hh == 0), stop=(hh == 7))
                P.V("tensor_tensor", out=tmpc[:, 0:N], in0=pa[:, 0:N], in1=gts[2][:, 0:N], op=ALU.mult)
                P.G("tensor_tensor", out=mT[:, dc, 0:N], in0=macc[:, 0:N], in1=tmpc[:, 0:N], op=ALU.add, pw=True)
            P.dma("sync", out=mTd.ap()[:, :, t0 * 128:t0 * 128 + N], in_=mT[:, :, 0:N])
        P.end_phase()
        if getattr(self, "mixw", None) is not None:
            P.span_end()
            self.mixw = None

    def phase_out(self, l, with_ctx):
        P = self.P
        self.moew = None
        if self.mode == "fused":
            P.span_begin()
            self.moew = dict(
                wgs=[P.sb_span(f"weg{i}", [128, 8, 512], BF16) for i in range(2)],
                wus=[P.sb_span(f"weu{i}", [128, 8, 512], BF16) for i in range(2)],
                wds=[P.sb_span(f"wed{i}", [128, 4, D], BF16) for i in range(2)])
        P.begin_phase()
        xsrc = self.xsrc(l)
        mTd = self.S(f"mT{l}", [128, 8, NTOK], BF16, False)
        xmid = self.S(f"xmid{l}", [NTOK, D], F32, True)
        h2d = self.S(f"h2T{l}", [128, 8, NTOK], BF16, True)
        pTs = [P.ps(f"pT{i}", [128, 8, 128], BF16) for i in range(2)]
        pB = [P.ps(f"pB{i}", [128, 1024], F32) for i in range(2)]
        pRs = [P.ps(f"pR{i}", [128, 512], F32) for i in range(2)]
        wo = P.sb("wo", [128, 8, D], BF16)
        P.dma("gpsimd", out=wo[:], in_=self.w_out.ap()[l].rearrange("(c p) n -> p c n", p=128))
        rw = P.sb("rw", [128, 8, NE], BF16)
        P.dma("gpsimd", out=rw[:], in_=self.router_w.ap().rearrange("(c p) n -> p c n", p=128))
        rb = P.sb("rb", [128, NE], F32)
        P.dma("sync", out=rb[:], in_=self.router_b.ap().partition_broadcast(128))
        if self.moew is not None:
            for e in range(2):
                P.dma("gpsimd", out=self.moew["wgs"][e][:], in_=self.w_e_gate.ap()[l, e].rearrange("(c p) n -> p c n", p=128))
                P.dma("gpsimd", out=self.moew["wus"][e][:], in_=self.w_e_up.ap()[l, e].rearrange("(c p) n -> p c n", p=128))
                P.dma("gpsimd", out=self.moew["wds"][e][:], in_=self.w_e_down.ap()[l, e].rearrange("(c p) n -> p c n", p=128))
        xts = [P.sb(f"xt{i}", [128, D], F32) for i in range(4)]
        xnews = [P.sb(f"xnew{i}", [128, D], F32) for i in range(4)]
        sqs = [P.sb(f"sq{i}", [128, D], BF16) for i in range(4)]
        xns = [P.sb(f"xn{i}", [128, D], BF16) for i in range(4)]
        mTs = [P.sb(f"mT{i}", [128, 8, 512], BF16) for i in range(2)]
        h2Ts = [P.sb(f"h2T{i}", [128, 8, 512], BF16) for i in range(2)]
        for bi, blk in enumerate(self.blocks(l, with_ctx)):
            nt = len(blk)
            N = nt * 128
            t0 = blk[0]
            cnd = 1 if t0 >= 32 else 0
            mT = mTs[bi % 2]
            h2T = h2Ts[bi % 2]
            pR = pRs[bi % 2]
            P.dma("sync", out=mT[:, :, 0:N], in_=mTd.ap()[:, :, t0 * 128:t0 * 128 + N])
            P.newgen(h2T)

            def sub(j, tile):
                js = slice(j * 128, (j + 1) * 128)
                pb = pB[j % 2]
                xt, xnew = xts[j], xnews[j]
                P.dma("sync", out=xt[:], in_=xsrc.ap()[tile * 128:(tile + 1) * 128, :])
                for half in range(2):
                    for dc in range(8):
                        P.T("matmul", out=pb[:, half * 512:(half + 1) * 512], lhsT=mT[:, dc, js], rhs=wo[:, dc, half * 512:(half + 1) * 512],
                            start=(dc == 0), stop=(dc == 7))
                P.V("tensor_tensor", out=xnew[:], in0=pb[:], in1=self.gbc[:, 0, cnd, :], op=ALU.mult)
                yield
                P.G("tensor_tensor", out=xnew[:], in0=xnew[:], in1=xt[:], op=ALU.add)
                yield
                P.dma("sync", out=xmid.ap()[tile * 128:(tile + 1) * 128, :], in_=xnew[:])
                st = P.scr("st", [128, 2], F32, nbuf=4)
                yield from self.gen_hT(xnew, lambda c, j=j, h2T=h2T: h2T[:, c, j * 128:(j + 1) * 128], self.a2, 24, cnd,
                                       (sqs[j], st, xns[j], pTs[j % 2]))
                for c in range(8):
                    P.T("matmul", out=pR[:, j * NE:(j + 1) * NE], lhsT=h2T[:, c, js], rhs=rw[:, c, :], start=(c == 0), stop=(c == 7))
                yield

            self.run_chains([sub(j, tile) for j, tile in enumerate(blk)])
            self.router_batch(pR[:, 0:nt * NE], rb, t0, nt)
            P.dma("sync", out=h2d.ap()[:, :, t0 * 128:t0 * 128 + N], in_=h2T[:, :, 0:N])
        P.end_phase()

    def router_batch(self, logits, rb, t0, S):
        P = self.P
        BIG = 1.0e4
        sc = P.scr("rb_sc", [128, 4, NE], F32)
        sel = P.scr("rb_sel", [128, 4, NE], F32)
        eq = P.scr("rb_eq", [128, 4, NE], F32)
        s2 = P.scr("rb_s2", [128, 4, NE], F32)
        selm = P.scr("rb_selm", [128, 4, NE], F32)
        e1 = P.scr("rb_e1", [128, 4, NE], F32)
        e2 = P.scr("rb_e2", [128, 4, NE], F32)
        m1 = P.scr("rb_m1", [128, 16], F32)
        m2 = P.scr("rb_m2", [128, 16], F32)
        ing = P.scr("rb_ing", [128, 16], F32)
        gm = P.scr("rb_gm", [128, 4, 4], F32)
        fl = lambda t: t[:, 0:S, :].rearrange("p s e -> p (s e)")
        g4 = lambda t: t[:, 0:S, :].rearrange("p s (g e) -> p (s g) e", e=4)
        P.A("activation", out=fl(sc), in_=logits, func=AF.Sigmoid)
        P.V("tensor_tensor", out=sel[:, 0:S, :], in0=sc[:, 0:S, :], in1=rb[:].unsqueeze(1).broadcast_to([128, S, NE]), op=ALU.add)
        P.V("tensor_reduce", out=m1[:, 0:S * 4], in_=g4(sel), axis=AX.X, op=ALU.max)
        P.V("tensor_tensor", out=g4(eq), in0=g4(sel), in1=m1[:, 0:S * 4].unsqueeze(2).broadcast_to([128, S * 4, 4]), op=ALU.is_equal)
        P.V("scalar_tensor_tensor", out=fl(s2), in0=fl(eq), scalar=-BIG, in1=fl(sel), op0=ALU.mult, op1=ALU.add)
        P.V("tensor_reduce", out=m2[:, 0:S * 4], in_=g4(s2), axis=AX.X, op=ALU.max)
        P.V("tensor_tensor", out=m1[:, 0:S * 4], in0=m1[:, 0:S * 4], in1=m2[:, 0:S * 4], op=ALU.add)
        grp = m1[:, 0:S * 4].rearrange("p (s g) -> p s g", g=4)
        P.V("tensor_reduce", out=gm[:, 0, 0:S], in_=grp, axis=AX.X, op=ALU.max)
        P.V("tensor_tensor", out=ing[:, 0:S * 4].rearrange("p (s g) -> p s g", g=4), in0=grp,
            in1=gm[:, 0, 0:S].unsqueeze(2).broadcast_to([128, S, 4]), op=ALU.is_equal)
        P.V("tensor_scalar", out=ing[:, 0:S * 4], in0=ing[:, 0:S * 4], scalar1=-1.0, scalar2=BIG, op0=ALU.add, op1=ALU.mult)
        P.V("tensor_tensor", out=g4(selm), in0=g4(sel), in1=ing[:, 0:S * 4].unsqueeze(2).broadcast_to([128, S * 4, 4]), op=ALU.add)
        P.V("tensor_reduce", out=gm[:, 1, 0:S], in_=selm[:, 0:S, :], axis=AX.X, op=ALU.max)
        P.V("tensor_tensor", out=e1[:, 0:S, :], in0=selm[:, 0:S, :], in1=gm[:, 1, 0:S].unsqueeze(2).broadcast_to([128, S, NE]), op=ALU.is_equal)
        P.V("scalar_tensor_tensor", out=fl(s2), in0=fl(e1), scalar=-BIG, in1=fl(selm), op0=ALU.mult, op1=ALU.add)
        P.V("tensor_reduce", out=gm[:, 2, 0:S], in_=s2[:, 0:S, :], axis=AX.X, op=ALU.max)
        P.V("tensor_tensor", out=e2[:, 0:S, :], in0=s2[:, 0:S, :], in1=gm[:, 2, 0:S].unsqueeze(2).broadcast_to([128, S, NE]), op=ALU.is_equal)
        P.V("tensor_tensor", out=fl(e1), in0=fl(e1), in1=fl(e2), op=ALU.add)
        P.V("tensor_tensor", out=fl(e1), in0=fl(e1), in1=fl(sc), op=ALU.mult)
        P.V("tensor_reduce", out=gm[:, 3, 0:S], in_=e1[:, 0:S, :], axis=AX.X, op=ALU.add)
        P.V("reciprocal", out=gm[:, 3, 0:S], in_=gm[:, 3, 0:S])
        P.V("tensor_tensor", out=self.gate_all[:, t0:t0 + S, :], in0=e1[:, 0:S, :],
            in1=gm[:, 3, 0:S].unsqueeze(2).broadcast_to([128, S, NE]), op=ALU.mult, pw=True)

    def router(self, logits, rb, tile):
        P = self.P
        BIG = 1.0e4
        sc = P.scr("r_sc", [128, NE], F32)
        sel = P.scr("r_sel", [128, 4, 4], F32)
        m1 = P.scr("r_m1", [128, 4], F32)
        eq = P.scr("r_eq", [128, 4, 4], F32)
        s2 = P.scr("r_s2", [128, 4, 4], F32)
        m2 = P.scr("r_m2", [128, 4], F32)
        gm = P.scr("r_gm", [128, 2], F32)
        ing = P.scr("r_ing", [128, 4], F32)
        e1 = P.scr("r_e1", [128, NE], F32)
        e2 = P.scr("r_e2", [128, NE], F32)
        selm = P.scr("r_selm", [128, 4, 4], F32)
        sel_f = sel[:].rearrange("p a b -> p (a b)")
        selm_f = selm[:].rearrange("p a b -> p (a b)")
        s2_f = s2[:].rearrange("p a b -> p (a b)")
        P.A("activation", out=sc[:], in_=logits, func=AF.Sigmoid)
        P.V("tensor_tensor", out=sel_f, in0=sc[:], in1=rb[:], op=ALU.add)
        P.V("tensor_reduce", out=m1[:], in_=sel[:], axis=AX.X, op=ALU.max)
        P.V("tensor_tensor", out=eq[:], in0=sel[:], in1=m1[:].unsqueeze(2).broadcast_to([128, 4, 4]), op=ALU.is_equal)
        P.V("scalar_tensor_tensor", out=s2[:], in0=eq[:], scalar=-BIG, in1=sel[:], op0=ALU.mult, op1=ALU.add)
        P.V("tensor_reduce", out=m2[:], in_=s2[:], axis=AX.X, op=ALU.max)
        P.V("tensor_tensor", out=m1[:], in0=m1[:], in1=m2[:], op=ALU.add)
        P.V("tensor_reduce", out=gm[:, 0:1], in_=m1[:], axis=AX.X, op=ALU.max)
        P.V("tensor_scalar", out=ing[:], in0=m1[:], scalar1=gm[:, 0:1], scalar2=None, op0=ALU.is_equal)
        P.V("tensor_scalar", out=ing[:], in0=ing[:], scalar1=-1.0, scalar2=BIG, op0=ALU.add, op1=ALU.mult)
        P.V("tensor_tensor", out=selm[:], in0=sel[:], in1=ing[:].unsqueeze(2).broadcast_to([128, 4, 4]), op=ALU.add)
        P.V("tensor_reduce", out=gm[:, 0:1], in_=selm_f, axis=AX.X, op=ALU.max)
        P.V("tensor_scalar", out=e1[:], in0=selm_f, scalar1=gm[:, 0:1], scalar2=None, op0=ALU.is_equal)
        P.V("scalar_tensor_tensor", out=s2_f, in0=e1[:], scalar=-BIG, in1=selm_f, op0=ALU.mult, op1=ALU.add)
        P.V("tensor_reduce", out=gm[:, 1:2], in_=s2_f, axis=AX.X, op=ALU.max)
        P.V("tensor_scalar", out=e2[:], in0=s2_f, scalar1=gm[:, 1:2], scalar2=None, op0=ALU.is_equal)
        P.V("tensor_tensor", out=e1[:], in0=e1[:], in1=e2[:], op=ALU.add)
        P.V("tensor_tensor", out=e1[:], in0=e1[:], in1=sc[:], op=ALU.mult)
        P.V("tensor_reduce", out=gm[:, 0:1], in_=e1[:], axis=AX.X, op=ALU.add)
        P.V("reciprocal", out=gm[:, 0:1], in_=gm[:, 0:1])
        P.V("tensor_scalar", out=self.gate_all[:, tile, :], in0=e1[:], scalar1=gm[:, 0:1], scalar2=None, op0=ALU.mult, pw=True)

    def phase_moe(self, l, with_ctx, final):
        P = self.P
        P.begin_phase()
        xmid = self.S(f"xmid{l}", [NTOK, D], F32, False)
        h2d = self.S(f"h2T{l}", [128, 8, NTOK], BF16, False)
        if final:
            xout = P.dram("out", [NLAT, D], F32, "ExternalOutput")
        else:
            xout = self.S("xres", [NTOK, D], F32, True)
        pG = [P.ps(f"pG{i}", [128, 512], F32) for i in range(2)]
        pU = [P.ps(f"pU{i}", [128, 512], F32) for i in range(2)]
        pY = [P.ps(f"pY{i}", [128, 1024], F32) for i in range(2)]
        tiles = list(range(NT if with_ctx else 32))
        groups = [g for g in (tiles[0:12], tiles[12:24], tiles[24:]) if g]
        pre = getattr(self, "moew", None)
        if pre is not None:
            wgs, wus, wds = pre["wgs"], pre["wus"], pre["wds"]
        else:
            wgs = [P.sb(f"weg{i}", [128, 8, 512], BF16) for i in range(2)]
            wus = [P.sb(f"weu{i}", [128, 8, 512], BF16) for i in range(2)]
            wds = [P.sb(f"wed{i}", [128, 4, D], BF16) for i in range(2)]
        h2 = P.sb("h2", [128, 8, 12 * 128], BF16)
        yacc = [P.sb(f"yacc{i}", [128, D], F32) for i in range(12)]
        aTs = [P.sb(f"aT{i}", [128, 4, 512], BF16) for i in range(2)]
        sgs = [P.sb(f"sg{i}", [128, 512], F32) for i in range(2)]
        xts = [P.sb(f"xm{i}", [128, D], F32) for i in range(2)]
        work = []
        for gi, hv in enumerate(groups):
            nth = len(hv)
            chunks = [list(range(i, min(i + 4, nth))) for i in range(0, nth, 4)]
            for e in range(NE):
                for ci, ch in enumerate(chunks):
                    work.append((gi, e, ch, ci == 0, ci == len(chunks) - 1))
        wslot = {}
        nload = [0]

        def load_w(gi, e):
            if (gi, e) in wslot or gi >= len(groups) or e >= NE:
                return
            sl = nload[0] % 2
            nload[0] += 1
            wslot[(gi, e)] = sl
            P.dma("gpsimd", out=wgs[sl][:], in_=self.w_e_gate.ap()[l, e].rearrange("(c p) n -> p c n", p=128))
            P.dma("gpsimd", out=wus[sl][:], in_=self.w_e_up.ap()[l, e].rearrange("(c p) n -> p c n", p=128))
            P.dma("gpsimd", out=wds[sl][:], in_=self.w_e_down.ap()[l, e].rearrange("(c p) n -> p c n", p=128))

        def nxt(gi, e):
            return (gi, e + 1) if e + 1 < NE else (gi + 1, 0)

        def emit_GU(k):
            gi, e, ch, first_ch, last_ch = work[k]
            hv = groups[gi]
            if e == 0 and first_ch:
                P.dma("sync", out=h2[:, :, 0:len(hv) * 128], in_=h2d.ap()[:, :, hv[0] * 128:(hv[0] + len(hv)) * 128])
            load_w(gi, e)
            sl = wslot[(gi, e)]
            N = len(ch) * 128
            cs = slice(ch[0] * 128, ch[0] * 128 + N)
            aT = aTs[k % 2]
            P.newgen(aT)
            for f in range(4):
                pg, pu, sg = pG[f % 2], pU[f % 2], sgs[f % 2]
                for c in range(8):
                    P.T("matmul", out=pg[:, 0:N], lhsT=wgs[sl][:, c, f * 128:(f + 1) * 128], rhs=h2[:, c, cs], start=(c == 0), stop=(c == 7))
                for c in range(8):
                    P.T("matmul", out=pu[:, 0:N], lhsT=wus[sl][:, c, f * 128:(f + 1) * 128], rhs=h2[:, c, cs], start=(c == 0), stop=(c == 7))
                P.A("activation", out=sg[:, 0:N], in_=pg[:, 0:N], func=AF.Silu)
                P.V("tensor_tensor", out=aT[:, f, 0:N], in0=pu[:, 0:N], in1=sg[:, 0:N], op=ALU.mult, pw=True)

        def emit_DOWN(k):
            gi, e, ch, first_ch, last_ch = work[k]
            hv = groups[gi]
            sl = wslot[(gi, e)]
            aT = aTs[k % 2]
            for jj, tl in enumerate(ch):
                py = pY[jj % 2]
                for half in range(2):
                    for f in range(4):
                        P.T("matmul", out=py[:, half * 512:(half + 1) * 512], lhsT=aT[:, f, jj * 128:(jj + 1) * 128],
                            rhs=wds[sl][:, f, half * 512:(half + 1) * 512], start=(f == 0), stop=(f == 3))
                gcol = self.gate_all[:, hv[0] + tl, e:e + 1]
                if e == 0:
                    P.V("tensor_scalar", out=yacc[tl][:], in0=py[:], scalar1=gcol, scalar2=None, op0=ALU.mult)
                else:
                    P.V("scalar_tensor_tensor", out=yacc[tl][:], in0=py[:], scalar=gcol, in1=yacc[tl][:], op0=ALU.mult, op1=ALU.add)
                if e == NE - 1:
                    emit_residual_tile(gi, tl)

        def emit_residual_tile(gi, tl):
            hv = groups[gi]
            if True:
                tile = hv[tl]
                cnd = 1 if tile >= 32 else 0
                xt = xts[tl % 2]
                P.dma("sync", out=xt[:], in_=xmid.ap()[tile * 128:(tile + 1) * 128, :])
                xo = P.scr("xo", [128, D], F32)
                P.G("tensor_tensor", out=xo[:], in0=yacc[tl][:], in1=self.gbc[:, 1, cnd, :], op=ALU.mult)
                P.G("tensor_tensor", out=xo[:], in0=xo[:], in1=xt[:], op=ALU.add)
                P.dma("sync", out=xout.ap()[tile * 128:(tile + 1) * 128, :], in_=xo[:], is_output=(final or self.mode != "fused"))

        nw = len(work)
        if pre is not None:
            wslot[(0, 0)] = 0
            wslot[(0, 1)] = 1
            nload[0] = 2
        else:
            load_w(0, 0)
            load_w(*nxt(0, 0))
        emit_GU(0)
        for k in range(nw):
            if k + 1 < nw:
                emit_GU(k + 1)
            emit_DOWN(k)
            gi, e, ch, first_ch, last_ch = work[k]
            if last_ch:
                load_w(*nxt(*nxt(gi, e)))

        P.end_phase(final=final)
        if pre is not None:
            P.span_end()
            self.moew = None


def build_program(mode, seg):
    B = Builder(mode, seg)
    P = B.P
    if mode == "fused":
        for l in range(2):
            last = (l == 1)
            B.phase_adaln(l)
            B.phase_p1(l)
            B.phase_attn(l, not last)
            B.phase_mix(l, not last)
            B.phase_out(l, not last)
            B.phase_moe(l, not last, last)
        nc = P.finish()
        return nc, list(P.ext_in), list(P.ext_out)
    if seg == 0:
        B.phase_adaln(0)
        B.phase_p1(0)
    elif seg == 1:
        B.phase_adaln(0)
        B.phase_attn(0, True)
        B.phase_mix(0, True)
        B.phase_out(0, True)
        B.phase_moe(0, True, False)
        B.phase_adaln(1)
        B.phase_p1(1)
    else:
        B.phase_adaln(1)
        B.phase_attn(1, False)
        B.phase_mix(1, False)
        B.phase_out(1, False)
        B.phase_moe(1, False, True)
    if seg != 2:
        P.begin_phase()
        P.end_phase(final=True)
    nc = P.finish()
    return nc, list(P.ext_in), list(P.ext_out)


def _rope_tables(s):
    pos = (s * NLAT + np.arange(NLAT)).astype(np.int64)
    row = (pos // 64).astype(np.float32)
    col = (pos % 64).astype(np.float32)
    inv = (np.float32(10000.0) ** (-(np.arange(8, dtype=np.float32) * np.float32(2.0) / np.float32(16.0)))).astype(np.float32)
    ar = (row[:, None] * inv).astype(np.float32)
    ac = (col[:, None] * inv).astype(np.float32)
    ang = np.concatenate([ar, ar, ac, ac], axis=-1)
    cos = np.cos(ang).astype(np.float32)
    sin = np.sin(ang).astype(np.float32)
    sgn = np.concatenate([-np.ones(8), np.ones(8), -np.ones(8), np.ones(8)]).astype(np.float32)
    t = np.zeros((NTOK, 2, 32), np.float32)
    t[:NLAT, 0] = cos
    t[:NLAT, 1] = sin * sgn
    t[NLAT:, 0] = 1.0
    return t


def _pool_coef(t, s, w, n):
    lo = max(t - w // 2, 0)
    hi = min(t + (w - w // 2), n)
    v = 0.0
    if lo <= s < hi:
        v += 1.0 / (hi - lo)
    if s == t:
        v -= 1.0
    return v


def _band(t_base, s_base, n, rows=128):
    out = np.zeros((4, 128, 128), np.float32)
    for g, w in enumerate((2, 4, 8, 16)):
        for tl in range(128):
            t = t_base + tl
            for s in range(max(t - 9, s_base), min(t + 9, s_base + rows)):
                if 0 <= s < n:
                    out[g, s - s_base, tl] = _pool_coef(t, s, w, n)
    return out


def _bands_universal():
    n = 8192
    mid = 10 * 128
    b = np.zeros((5, 4, 128, 128), np.float32)
    b[0] = _band(mid, mid, n)
    b[1] = _band(mid, mid - 128, n)
    b[2] = _band(mid, mid + 128, n)
    b[3] = _band(0, 0, n)
    b[4] = _band(n - 128, n - 128, n)
    return b


def _bands_core(s, uni):
    n = 8192
    b = np.zeros((4, 4, 128, 128), np.float32)
    if s == 0:
        b[0] = uni[3]
        b[1] = uni[0]
        e = _band(3968, 4096, n, rows=8)
        b[3][:, 16:24, :] = e[:, 0:8, :]
    else:
        b[0] = uni[0]
        b[1] = uni[4]
        e = _band(4096, 4088, n, rows=8)
        b[2][:, 8:16, :] = e[:, 0:8, :]
    return b


_CACHE = {}
FUSED = True
_LAST_STATE = None


def _get_prog(mode, seg):
    key = (mode, seg)
    if key not in _CACHE:
        _CACHE[key] = build_program(mode, seg)
    return _CACHE[key]


def kernel(**inp):
    inp = {k: np.ascontiguousarray(np.asarray(v)) for k, v in inp.items()}
    x, c, ctx, c_ctx = inp["x"], inp["c"], inp["ctx"], inp["c_ctx"]
    uni = _bands_universal()
    wnames = ["w_mod", "b_mod", "norm1_g", "norm2_g", "w_in", "pool_w", "pool_scale", "sgu_norm_g", "sgu_ws", "sgu_b",
              "qa_norm_g", "w_uq", "kva_norm_g", "w_ukv", "q_norm_g", "k_norm_g", "w_br_pool", "w_br_sgu", "w_br_mla",
              "b_gate", "w_out", "router_w", "router_b", "w_e_gate", "w_e_up", "w_e_down"]
    base = []
    for core in range(8):
        b, s = core // 2, core % 2
        m = {k: inp[k] for k in wnames}
        m["xin"] = np.concatenate([x[b, s * NLAT:(s + 1) * NLAT], ctx[b]], axis=0)
        m["cond"] = np.stack([c[b], c_ctx], axis=0)
        m["ropet"] = _rope_tables(s)
        m["bands"] = uni
        m["bandcore"] = _bands_core(s, uni)
        base.append(m)
    if FUSED:
        nc, ext_in, ext_out = _get_prog("fused", 0)
        in_maps = [{n: base[core][n] for n in ext_in} for core in range(8)]
        res = run_bass_kernel_spmd(nc, in_maps, core_ids=list(range(8)))
        out = np.zeros((4, 8192, D), np.float32)
        for core in range(8):
            b, s = core // 2, core % 2
            out[b, s * NLAT:(s + 1) * NLAT] = np.asarray(res.results[core]["out"])
        return out
    state = [dict() for _ in range(8)]
    global _LAST_STATE
    _LAST_STATE = state
    out = None
    for seg in range(3):
        nc, ext_in, ext_out = _get_prog("seg", seg)
        in_maps = []
        for core in range(8):
            mm = {}
            for n in ext_in:
                if n in base[core]:
                    mm[n] = base[core][n]
                else:
                    mm[n] = state[core][n]
            in_maps.append(mm)
        res = run_bass_kernel_spmd(nc, in_maps, core_ids=list(range(8)))
        for core in range(8):
            for n in ext_out:
                state[core][n] = np.asarray(res.results[core][n])
        for nm in ext_out:
            if "_own" in nm:
                for pair in range(4):
                    c0, c1 = 2 * pair, 2 * pair + 1
                    allv = np.stack([state[c0][nm], state[c1][nm]], axis=0)
                    state[c0][nm.replace("_own", "_all")] = allv
                    state[c1][nm.replace("_own", "_all")] = allv
        if "out" in ext_out:
            out = np.zeros((4, 8192, D), np.float32)
            for core in range(8):
                b, s = core // 2, core % 2
                out[b, s * NLAT:(s + 1) * NLAT] = state[core]["out"]
    return out
```

```python
import contextlib
import numpy as np
import concourse.bass as bass
import concourse.mybir as mybir
from concourse.bass_utils import run_bass_kernel_spmd

F32 = mybir.dt.float32
BF16 = mybir.dt.bfloat16
AF = mybir.ActivationFunctionType
ALU = mybir.AluOpType
AX = mybir.AxisListType

D = 1024
NLAT = 4096
NCTX = 256
NTOK = NLAT + NCTX
NT = NTOK // 128
OFF_U, OFF_V, OFF_QA, OFF_KVA, OFF_KR, OFF_GATE = 256, 512, 768, 1152, 1408, 1440
IN_W = 4512
EPS = 1e-6
SM_SCALE = 96 ** -0.5
NE = 16


class Trk:
    def __init__(self, handle, name):
        self.h = handle
        self.name = name
        self.writes = []
        self.reads = []
        self.war = []

    def __getitem__(self, idx):
        return self.h[idx]

    def ap(self):
        return self.h.ap()


def _compress(evs):
    best = {}
    for s, v in evs:
        k = id(s)
        if k not in best or best[k][1] < v:
            best[k] = (s, v)
    return list(best.values())


class Prog:
    NQ = 12
    WKEYS = ("out", "accum_out")

    def __init__(self):
        self.nc = bass.Bass("TRN2", target_bir_lowering=False)
        self.stack = contextlib.ExitStack()
        self.ops = {k: [] for k in ("tensor", "vector", "scalar", "gpsimd", "sync")}
        self.esem = {}
        self.ecount = {k: 0 for k in self.ops}
        self.known = {k: {} for k in self.ops}
        for k in self.ops:
            self.esem[k] = self.stack.enter_context(self.nc.semaphore("s_" + k))
        self.qsems, self.qn, self.quse = {}, {}, {}
        for q in ("sync", "gpsimd", "scalar"):
            self.qsems[q] = [self.stack.enter_context(self.nc.semaphore(f"d_{q}{i}")) for i in range(self.NQ)]
            self.qn[q] = 0
            self.quse[q] = [0] * self.NQ
        self.ccsem = self.stack.enter_context(self.nc.semaphore("cc_sem"))
        self.cccount = 0
        self.nt = 0
        self.out_events = []
        self.pstack = None
        self.reg = {}
        self.drams = {}
        self.ext_in = []
        self.ext_out = []

    def begin_phase(self):
        assert self.pstack is None
        self.pstack = contextlib.ExitStack()
        self.scrpool = {}

    def end_phase(self, final=False):
        nc = self.nc
        for q in self.qsems:
            for i, sem in enumerate(self.qsems[q]):
                if self.quse[q][i]:
                    self._wait("sync", (sem, self.quse[q][i] * 16))
        if self.cccount:
            self._wait("sync", (self.ccsem, self.cccount))
        if final:
            for ev in self.out_events:
                self._wait("sync", ev)
        with nc.Block() as block:
            def emit(engname):
                def body(e):
                    for o in self.ops[engname]:
                        if o[0] == "wait":
                            e.wait_ge(o[1], o[2])
                        elif o[0] == "op":
                            o[1](e).then_inc(o[2], 1)
                        elif o[0] == "cc":
                            _, in_ap, out_ap, sem, rg = o
                            e.collective_compute("AllGather", ALU.bypass, replica_groups=rg, ins=[in_ap], outs=[out_ap]).then_inc(sem, 1)
                        else:
                            _, out_ap, in_ap, sem, kw = o
                            e.dma_start(out=out_ap, in_=in_ap, **kw).then_inc(sem, 16)
                return body
            for k, regf in (("sync", block.sync), ("scalar", block.scalar), ("vector", block.vector),
                            ("gpsimd", block.gpsimd), ("tensor", block.tensor)):
                if self.ops[k]:
                    regf(emit(k))
        for k in self.ops:
            self.ops[k] = []
        self.pstack.close()
        self.pstack = None

    def finish(self):
        self.stack.close()
        return self.nc

    def dram(self, name, shape, dtype, kind):
        if name in self.drams:
            return self.drams[name]
        t = Trk(self.nc.dram_tensor(name, list(shape), dtype, kind=kind), name)
        t.kind = kind
        self.drams[name] = t
        self.reg[name] = t
        if kind == "ExternalInput":
            self.ext_in.append(name)
        elif kind == "ExternalOutput":
            self.ext_out.append(name)
        return t

    def span_begin(self):
        assert self.pstack is None and getattr(self, "sstack", None) is None
        self.sstack = contextlib.ExitStack()

    def span_end(self):
        assert self.pstack is None
        self.sstack.close()
        self.sstack = None

    def sb_span(self, name, shape, dtype=F32):
        self.nt += 1
        nm = f"{name}_{self.nt}"
        h = self.sstack.enter_context(self.nc.sbuf_tensor(nm, list(shape), dtype))
        t = Trk(h, nm)
        self.reg[nm] = t
        return t

    def _alloc(self, fn, name, shape, dtype, top):
        self.nt += 1
        nm = f"{name}_{self.nt}"
        st = self.stack if (top or self.pstack is None) else self.pstack
        h = st.enter_context(fn(nm, list(shape), dtype))
        t = Trk(h, nm)
        self.reg[nm] = t
        return t

    def sb(self, name, shape, dtype=F32, top=False):
        return self._alloc(self.nc.sbuf_tensor, name, shape, dtype, top)

    def scr(self, name, shape, dtype=F32, nbuf=2):
        key = (name, tuple(shape), str(dtype))
        pool = self.scrpool.setdefault(key, [[], 0])
        if len(pool[0]) < nbuf:
            t = self.sb(name, shape, dtype)
            pool[0].append(t)
            return t
        t = pool[0][pool[1] % nbuf]
        pool[1] += 1
        return t

    def ps(self, name, shape, dtype=F32, top=False):
        t = self._alloc(self.nc.psum_tensor, name, shape, dtype, top)
        t.psum = True
        return t

    def _wait(self, eng, ev):
        sem, val = ev
        kid = id(sem)
        kn = self.known[eng]
        if kn.get(kid, 0) >= val:
            return
        kn[kid] = val
        self.ops[eng].append(("wait", sem, val))

    def newgen(self, t):
        t.war = _compress(t.reads + t.writes)
        t.reads = []
        t.writes = []

    def _deps(self, eng, reads, writes, pw, skip_own=None):
        def w(ev):
            if skip_own is not None and ev[0] is skip_own:
                return
            self._wait(eng, ev)
        for t in reads:
            for ev in t.writes:
                w(ev)
        for t in writes:
            if pw:
                for ev in t.war:
                    w(ev)
                for ev in t.reads:
                    w(ev)
            else:
                for ev in t.writes:
                    w(ev)
                for ev in t.reads:
                    w(ev)
                for ev in t.war:
                    w(ev)

    def _commit(self, ev, reads, writes, pw):
        for t in reads:
            if t in writes:
                continue
            t.reads.append(ev)
            if len(t.reads) > 16:
                t.reads = _compress(t.reads)
        for t in writes:
            if pw:
                t.writes.append(ev)
                if len(t.writes) > 16:
                    t.writes = _compress(t.writes)
            else:
                t.writes = [ev]
                t.reads = []
                t.war = []

    def trk(self, ap):
        return self.reg[ap.tensor.name]

    def E(self, eng, name, pw=False, **kw):
        reads, writes = [], []
        for k, v in kw.items():
            if isinstance(v, bass.AP):
                (writes if k in self.WKEYS else reads).append(self.trk(v))
        self._deps(eng, reads, writes, pw, skip_own=self.esem[eng] if eng == "tensor" else None)
        if eng != "tensor":
            for t in reads:
                if getattr(t, "psum", False):
                    for ev in t.reads:
                        if ev[0] is not self.esem[eng]:
                            self._wait(eng, ev)
        self.ecount[eng] += 1
        ev = (self.esem[eng], self.ecount[eng])
        self.ops[eng].append(("op", (lambda e, name=name, kw=kw: getattr(e, name)(**kw)), self.esem[eng]))
        self._commit(ev, reads, writes, pw)
        return ev

    def dma(self, q, out, in_, is_output=False, pw=False, **kw):
        out_t, in_t = self.trk(out), self.trk(in_)
        i = self.qn[q] % self.NQ
        self.qn[q] += 1
        sem = self.qsems[q][i]
        prev = self.quse[q][i] * 16
        if prev:
            self._wait(q, (sem, prev))
        self._deps(q, [in_t], [out_t], pw)
        self.quse[q][i] += 1
        ev = (sem, self.quse[q][i] * 16)
        self.ops[q].append(("dma", out, in_, sem, kw))
        self._commit(ev, [in_t], [out_t], pw)
        if is_output or getattr(out_t, "kind", None) == "ExternalOutput":
            self.out_events.append(ev)
            if len(self.out_events) > 64:
                self.out_events = _compress(self.out_events)
        return ev

    def collective_toplevel(self, src, dst, src_ap, dst_ap, rg):
        assert self.pstack is None
        q = "gpsimd"
        i = self.qn[q] % self.NQ
        self.qn[q] += 1
        sem = self.qsems[q][i]
        prev = self.quse[q][i] * 16
        if prev:
            self._wait(q, (sem, prev))
        self._deps(q, [src], [dst], False)
        self.quse[q][i] += 1
        ev = (sem, self.quse[q][i] * 16)
        e = self.nc.gpsimd
        for o in self.ops[q]:
            assert o[0] == "wait"
            e.wait_ge(o[1], o[2])
        self.ops[q] = []
        e.collective_compute("AllGather", ALU.bypass, replica_groups=rg, ins=[src_ap], outs=[dst_ap]).then_inc(sem, 16)
        self._commit(ev, [src], [dst], False)
        return ev

    def V(self, name, **kw):
        return self.E("vector", name, **kw)

    def A(self, name, **kw):
        return self.E("scalar", name, **kw)

    def G(self, name, **kw):
        return self.E("gpsimd", name, **kw)

    def T(self, name, **kw):
        return self.E("tensor", name, **kw)


class Builder:
    def __init__(self, mode, seg):
        self.P = Prog()
        self.mode = mode
        self.seg = seg
        self.produced = set()
        P = self.P
        self.setup_consts()

    INPUTS = {
        "xin": [NTOK, D], "cond": [2, D], "w_mod": [2, D, 6 * D], "b_mod": [2, 6 * D], "norm1_g": [2, D], "norm2_g": [2, D],
        "w_in": [2, D, IN_W], "pool_w": [2, 4, 64, 64], "pool_scale": [2, 256], "sgu_norm_g": [2, 256],
        "sgu_ws": [2, 4, 128, 128], "sgu_b": [2, 4, 128], "qa_norm_g": [2, 384], "w_uq": [2, 384, 768],
        "kva_norm_g": [2, 256], "w_ukv": [2, 256, 1024], "q_norm_g": [2, 96], "k_norm_g": [2, 96],
        "w_br_pool": [2, 256, D], "w_br_sgu": [2, 256, D], "w_br_mla": [2, 512, D], "b_gate": [2, 3 * D],
        "w_out": [2, D, D], "router_w": [D, NE], "router_b": [NE], "w_e_gate": [2, NE, D, 512],
        "w_e_up": [2, NE, D, 512], "w_e_down": [2, NE, 512, D], "ropet": [NTOK, 2, 32],
        "bands": [5, 4, 128, 128], "bandcore": [4, 4, 128, 128],
    }

    def __getattr__(self, name):
        if name in Builder.INPUTS:
            t = self.P.dram(name, Builder.INPUTS[name], F32, "ExternalInput")
            return t
        raise AttributeError(name)

    def S(self, name, shape, dtype, write):
        P = self.P
        if name in P.drams:
            return P.drams[name]
        if self.mode == "fused":
            kind = "Internal"
        else:
            kind = "ExternalOutput" if write else "ExternalInput"
        return P.dram(name, shape, dtype, kind)

    def setup_consts(self):
        P = self.P
        P.begin_phase()
        self.idf = P.sb("idf", [128, 128], F32, top=True)
        self.ident = P.sb("ident", [128, 128], BF16, top=True)
        self.ones_b = P.sb("ones_b", [128, 2], BF16, top=True)
        self.ones_f = P.sb("ones_f", [128, 128], F32, top=True)
        self.memset(self.idf, self.idf[:], 1.0, eng="gpsimd")
        P.E("gpsimd", "affine_select", out=self.idf[:], in_=self.idf[:], pattern=[[-1, 128]],
            compare_op=ALU.is_equal, fill=0.0, base=0, channel_multiplier=1)
        P.V("tensor_copy", out=self.ident[:], in_=self.idf[:])
        self.memset(self.ones_b, self.ones_b[:], 1.0)
        self.memset(self.ones_f, self.ones_f[:], 1.0)
        self.modfm = P.sb("modfm", [128, 48, 2], F32, top=True)
        self.a1 = P.sb("a1", [128, 8, 2], F32, top=True)
        self.a2 = P.sb("a2", [128, 8, 2], F32, top=True)
        self.gbc = P.sb("gbc", [128, 2, 2, D], F32, top=True)
        self.gate_all = P.sb("gate_all", [128, NT, NE], F32, top=True)
        P.end_phase()

    def _mark_write(self, t):
        pass

    def memset(self, t, ap, val, eng="vector"):
        P = self.P
        reads, writes = [], [t]
        P._deps(eng, reads, writes, False)
        P.ecount[eng] += 1
        ev = (P.esem[eng], P.ecount[eng])
        P.ops[eng].append(("op", (lambda e, ap=ap, val=val: e.memset(ap, val)), P.esem[eng]))
        P._commit(ev, reads, writes, False)

    def load_fm(self, dst_ap, src_ap, n, rows=128):
        P = self.P
        k = n // rows
        tmp = P.sb("lfm", [k, rows], F32)
        P.dma("sync", out=tmp[:], in_=src_ap.rearrange("(j p) -> j p", p=rows))
        pt = self.ps_misc
        P.T("transpose", out=pt[0:rows, 0:k], in_=tmp[:], identity=self.idf[0:k, 0:k])
        P.V("tensor_copy", out=dst_ap, in_=pt[0:rows, 0:k])

    def rstd(self, dst, src, n, cols):
        P = self.P
        P.A("activation", out=dst, in_=src, func=AF.Sqrt, scale=1.0 / n, bias=EPS)
        P.V("reciprocal", out=dst, in_=dst)

    def phase_adaln(self, l):
        P = self.P
        P.begin_phase()
        self.ps_misc = P.ps("ps_misc", [128, 512], F32)
        psm = P.ps("psm", [128, 96], F32)
        prow = [P.ps(f"prow{i}", [128, 512], F32) for i in range(4)]
        csb = P.sb("csb", [2, D], F32)
        P.dma("sync", out=csb[:], in_=self.cond.ap())
        pc = self.ps_misc
        for j in range(8):
            P.T("transpose", out=pc[:, 2 * j:2 * j + 2], in_=csb[0:2, j * 128:(j + 1) * 128], identity=self.idf[0:2, 0:2])
        scT = P.sb("scT", [128, 8, 2], BF16)
        P.A("activation", out=scT[:].rearrange("p a b -> p (a b)"), in_=pc[:, 0:16], func=AF.Silu)
        bm = P.sb("bm", [1, 6 * D], BF16)
        P.dma("gpsimd", out=bm[:], in_=self.b_mod.ap()[l:l + 1, :])
        n1 = P.sb("n1", [128, 8], F32)
        n2 = P.sb("n2", [128, 8], F32)
        self.load_fm(n1[:], self.norm1_g.ap()[l], D)
        self.load_fm(n2[:], self.norm2_g.ap()[l], D)
        wm = [P.sb(f"wm{i}", [128, 6 * D], BF16) for i in range(2)]
        gcols = [2 * D, 2 * D + 512, 5 * D, 5 * D + 512]
        macc = P.sb("modacc", [128, 96], F32)
        psms = [psm, P.ps("psm2", [128, 96], F32)]
        for c in range(8):
            w = wm[c % 2]
            pm_ = psms[c % 2]
            P.dma("gpsimd", out=w[:], in_=self.w_mod.ap()[l, c * 128:(c + 1) * 128, :])
            for j in range(48):
                P.T("matmul", out=pm_[:, 2 * j:2 * j + 2], lhsT=w[:, j * 128:(j + 1) * 128], rhs=scT[:, c, :],
                    start=True, stop=True)
            if c == 0:
                P.V("tensor_copy", out=macc[:], in_=pm_[:, 0:96])
            else:
                P.V("tensor_tensor", out=macc[:], in0=macc[:], in1=pm_[:, 0:96], op=ALU.add)
            for gi, gc in enumerate(gcols):
                P.T("matmul", out=prow[gi][0:2, :], lhsT=scT[:, c, :], rhs=w[:, gc:gc + 512], start=(c == 0), stop=False)
        pm_ = psms[0]
        for j in range(48):
            P.T("matmul", out=pm_[:, 2 * j:2 * j + 2], lhsT=bm[0:1, j * 128:(j + 1) * 128], rhs=self.ones_b[0:1, 0:2],
                start=True, stop=True)
        for gi, gc in enumerate(gcols):
            P.T("matmul", out=prow[gi][0:2, :], lhsT=self.ones_b[0:1, 0:2], rhs=bm[0:1, gc:gc + 512], start=False, stop=True)
        P.V("tensor_tensor", out=self.modfm[:].rearrange("p a b -> p (a b)"), in0=macc[:], in1=pm_[:, 0:96], op=ALU.add)
        for (a, n, off) in ((self.a1, n1, 8), (self.a2, n2, 32)):
            P.V("tensor_scalar", out=a[:], in0=self.modfm[:, off:off + 8, :], scalar1=1.0, scalar2=None, op0=ALU.add)
            P.V("tensor_tensor", out=a[:], in0=a[:], in1=n[:].unsqueeze(2).broadcast_to([128, 8, 2]), op=ALU.mult)
        grow = P.sb("grow", [2, 2048], F32)
        for gi in range(4):
            P.V("tensor_copy", out=grow[0:2, gi * 512:(gi + 1) * 512], in_=prow[gi][0:2, :])
        gd = self.P.dram(f"growd{l}_{self.seg}", [2, 2048], F32, "Internal")
        P.dma("sync", out=gd.ap(), in_=grow[:])
        for gsel in range(2):
            for cnd in range(2):
                P.dma("sync", out=self.gbc[:, gsel, cnd, :],
                      in_=gd.ap()[cnd, gsel * D:(gsel + 1) * D].partition_broadcast(128))
        P.end_phase()

    def gen_hT(self, xt, hT_ap_fn, a, shoff, cnd, gamma_scr):
        P = self.P
        sq, st, xn, pT = gamma_scr
        P.A("activation", out=sq[:], in_=xt[:], func=AF.Square, accum_out=st[:, 0:1])
        yield
        P.A("activation", out=st[:, 1:2], in_=st[:, 0:1], func=AF.Sqrt, scale=1.0 / D, bias=EPS)
        yield
        P.V("reciprocal", out=st[:, 1:2], in_=st[:, 1:2])
        yield
        P.V("tensor_scalar", out=xn[:], in0=xt[:], scalar1=st[:, 1:2], scalar2=None, op0=ALU.mult)
        yield
        for c in range(8):
            P.T("transpose", out=pT[:, c, :], in_=xn[:, c * 128:(c + 1) * 128], identity=self.ident[:])
        for c in range(8):
            P.A("activation", out=hT_ap_fn(c), in_=pT[:, c, :], func=AF.Identity,
                scale=a[:, c, cnd:cnd + 1], bias=self.modfm[:, shoff + c, cnd:cnd + 1], pw=True)
        yield

    def emit_hT(self, xt, hT_ap_fn, a, shoff, cnd, gamma_scr):
        for _ in self.gen_hT(xt, hT_ap_fn, a, shoff, cnd, gamma_scr):
            pass

    @staticmethod
    def run_chains(chains):
        chains = list(chains)
        while chains:
            nxt = []
            for g in chains:
                try:
                    next(g)
                    nxt.append(g)
                except StopIteration:
                    pass
            chains = nxt

    def blocks(self, l, with_ctx, size=4):
        bl = [list(range(i, i + size)) for i in range(0, 32, size)]
        if with_ctx:
            bl.append([32, 33])
        return bl

    def xsrc(self, l):
        if l == 0:
            return self.xin
        return self.S("xres", [NTOK, D], F32, write=False)

    def phase_p1(self, l):
        P = self.P
        P.begin_phase()
        xsrc = self.xsrc(l)
        qx = self.S(f"qx{l}", [96, 8, NTOK], BF16, True)
        kxo = [self.S(f"kx_own{l}_{c}", [2, 96, NTOK], BF16, True) for c in range(4)]
        vxo = [self.S(f"vx_own{l}_{c}", [2, 128, NT, 65], BF16, True) for c in range(4)]
        pall_d = self.S(f"pall{l}", [128, NT, 256], BF16, True)
        pe = self.S(f"pe_own{l}", [16, 256], BF16, True)
        pT = [P.ps("pT0", [128, 8, 128], BF16)] * 2
        pfm = [P.ps("pfm0", [128, 512], F32)] * 2
        ptm = P.ps("ptm", [128, 1024], F32)
        ptm2 = P.ps("ptm2", [128, 1024], F32)
        psmall = P.ps("psmall", [128, 512], F32)
        self.ps_misc = psmall
        ptr = P.ps("ptr", [128, 8, 128], BF16)
        ptr2 = ptr
        w1 = P.sb("w1", [128, 8, 672], BF16)
        wp = P.sb("wp", [128, 8, 256], BF16)
        win = self.w_in.ap()[l].rearrange("(c p) n -> p c n", p=128)
        P.dma("gpsimd", out=w1[:], in_=win[:, :, OFF_QA:OFF_GATE])
        P.dma("gpsimd", out=wp[:], in_=win[:, :, 0:256])
        wuq = P.sb("wuq", [128, 3, 768], BF16)
        P.dma("gpsimd", out=wuq[:], in_=self.w_uq.ap()[l].rearrange("(c p) n -> p c n", p=128))
        wukv = P.sb("wukv", [128, 2, 1024], BF16)
        P.dma("gpsimd", out=wukv[:], in_=self.w_ukv.ap()[l].rearrange("(c p) n -> p c n", p=128))
        gfm = P.sb("gfm", [128, 5], F32)
        self.load_fm(gfm[:, 0:3], self.qa_norm_g.ap()[l], 384)
        self.load_fm(gfm[:, 3:5], self.kva_norm_g.ap()[l], 256)
        qg = P.sb("qg", [128, 96], F32)
        kg = P.sb("kg", [128, 96], F32)
        P.dma("sync", out=qg[:], in_=self.q_norm_g.ap()[l].partition_broadcast(128))
        P.dma("sync", out=kg[:], in_=self.k_norm_g.ap()[l].partition_broadcast(128))
        xts = [P.sb(f"xt{i}", [128, D], F32) for i in range(2)]
        sq = P.sb("sq", [128, D], BF16)
        xn = P.sb("xn", [128, D], BF16)
        hTs = [P.sb(f"hT{i}", [128, 8, 512], BF16) for i in range(2)]
        sq_b = P.sb("sq_b", [128, D], BF16)
        xn_b = P.sb("xn_b", [128, D], BF16)
        sqT = P.sb("sqT", [128, 5, 512], BF16)
        aT = P.sb("aT", [128, 5, 512], BF16)
        p_all = P.sb("p_all", [128, NT, 256], BF16)
        Qblk = [P.sb("Qblk0", [96, 8, 512], BF16)]
        Kblk = [P.sb("Kblk0", [96, 8, 512], BF16)]
        Vblk = [P.sb(f"Vblk{i}", [128, 8, 4, 65], BF16) for i in range(2)]
        for v in Vblk:
            self.memset(v, v[:], 1.0)
        rt = [P.sb(f"rt{i}", [128, 2, 32], F32) for i in range(4)]
        ti = 0
        stop = getattr(self, "stop", 99)
        blks_all = self.blocks(l, True)

        def hT_gens(bi):
            blk = blks_all[bi]
            hT = hTs[bi % 2]
            P.newgen(hT)

            def gen(which):
                scr_ = (sq, xn) if which == 0 else (sq_b, xn_b)
                xt = xts[which]
                for j, tile in enumerate(blk):
                    if j % 2 != which:
                        continue
                    cnd = 1 if tile >= 32 else 0
                    P.dma("sync", out=xt[:], in_=xsrc.ap()[tile * 128:(tile + 1) * 128, :])
                    st = P.scr("st", [128, 2], F32, nbuf=4)
                    yield from self.gen_hT(xt, lambda c, hT=hT, j=j: hT[:, c, j * 128:(j + 1) * 128], self.a1, 0, cnd,
                                           (scr_[0], st, scr_[1], pT[0]))
            return [gen(0), gen(1)]

        if stop > 1:
            self.run_chains(hT_gens(0))
        for bi, blk in enumerate(blks_all if stop > 1 else []):
            nt = len(blk)
            N = nt * 128
            hT = hTs[bi % 2]
            if stop <= 2:
                continue
            P.newgen(sqT)
            P.newgen(aT)
            for ci in range(5):
                pf = pfm[ci % 2]
                for c in range(8):
                    P.T("matmul", out=pf[:, 0:N], lhsT=w1[:, c, ci * 128:(ci + 1) * 128], rhs=hT[:, c, 0:N],
                        start=(c == 0), stop=(c == 7))
                P.A("activation", out=sqT[:, ci, 0:N], in_=pf[:, 0:N], func=AF.Square, pw=True)
                P.V("tensor_scalar", out=aT[:, ci, 0:N], in0=pf[:, 0:N], scalar1=gfm[:, ci:ci + 1], scalar2=None,
                    op0=ALU.mult, pw=True)
            if stop <= 3:
                continue
            Qb_, Kb_, Vb_ = Qblk[0], Kblk[0], Vblk[bi % 2]
            P.newgen(Qb_)
            P.newgen(Kb_)
            t0 = blk[0]
            chains = []
            for j, tile in enumerate(blk):
                js = slice(j * 128, (j + 1) * 128)
                rtt = rt[j % 4]
                P.dma("sync", out=rtt[:], in_=self.ropet.ap()[tile * 128:(tile + 1) * 128])
                for ci in range(3):
                    P.T("matmul", out=psmall[:, 0:1], lhsT=sqT[:, ci, js], rhs=self.ones_b[:, 0:1], start=(ci == 0), stop=(ci == 2))
                for ci in range(2):
                    P.T("matmul", out=psmall[:, 1:2], lhsT=sqT[:, 3 + ci, js], rhs=self.ones_b[:, 0:1], start=(ci == 0), stop=(ci == 1))
                for c in range(8):
                    P.T("matmul", out=psmall[:, 8:40], lhsT=hT[:, c, js], rhs=w1[:, c, 640:672], start=(c == 0), stop=(c == 7))
                need_p = not (l == 1 and tile >= 32)
                if need_p:
                    for c in range(8):
                        P.T("matmul", out=psmall[:, 64:320], lhsT=hT[:, c, js], rhs=wp[:, c, :], start=(c == 0), stop=(c == 7))
                st = P.scr("stq", [128, 8], F32, nbuf=4)
                junk = P.scr("junk", [128, 32], F32, nbuf=4)
                kr = P.scr("kr", [128, 32], F32, nbuf=4)
                P.A("activation", out=st[:, 0:1], in_=psmall[:, 0:1], func=AF.Sqrt, scale=1.0 / 384, bias=EPS)
                P.A("activation", out=st[:, 1:2], in_=psmall[:, 1:2], func=AF.Sqrt, scale=1.0 / 256, bias=EPS)
                P.A("activation", out=junk[:], in_=psmall[:, 8:40], func=AF.Square, accum_out=st[:, 2:3])
                P.A("activation", out=st[:, 3:4], in_=st[:, 2:3], func=AF.Sqrt, scale=1.0 / 32, bias=EPS)
                P.V("reciprocal", out=st[:, 4:8], in_=st[:, 0:4])
                rq, rkv, rkr = st[:, 4:5], st[:, 5:6], st[:, 7:8]
                if need_p:
                    P.A("activation", out=p_all[:, tile, :], in_=psmall[:, 64:320], func=AF.Identity, pw=True)
                P.A("activation", out=kr[:], in_=psmall[:, 8:40], func=AF.Identity, scale=rkr)
                need_q = not (l == 1 and tile >= 32)
                qs = P.scr("qs", [128, 8, 96], F32, nbuf=4)
                if need_q:
                    for ci in range(3):
                        P.T("matmul", out=ptm[:, 0:512], lhsT=aT[:, ci, js], rhs=wuq[:, ci, 0:512], start=(ci == 0), stop=(ci == 2))
                    for ci in range(3):
                        P.T("matmul", out=ptm[:, 512:768], lhsT=aT[:, ci, js], rhs=wuq[:, ci, 512:768], start=(ci == 0), stop=(ci == 2))
                    P.A("activation", out=qs[:].rearrange("p a b -> p (a b)"), in_=ptm[:, 0:768], func=AF.Identity, scale=rq)
                ptk = ptm2
                for half in range(2):
                    for ci in range(2):
                        P.T("matmul", out=ptk[:, half * 512:(half + 1) * 512], lhsT=aT[:, 3 + ci, js],
                            rhs=wukv[:, ci, half * 512:(half + 1) * 512], start=(ci == 0), stop=(ci == 1))
                ks = P.scr("ks", [128, 8, 128], F32, nbuf=4)
                P.A("activation", out=ks[:].rearrange("p a b -> p (a b)"), in_=ptk[:, 0:1024], func=AF.Identity, scale=rkv)

                def chain_q(j=j, js=js, qs=qs, rtt=rtt):
                    sq2 = P.scr("sq2", [128, 8, 96], F32, nbuf=4)
                    st2 = P.scr("st2", [128, 16], F32, nbuf=4)
                    P.G("tensor_tensor", out=sq2[:], in0=qs[:], in1=qs[:], op=ALU.mult); yield
                    P.V("tensor_reduce", out=st2[:, 0:8], in_=sq2[:, :, 0:64], axis=AX.X, op=ALU.add); yield
                    P.V("tensor_reduce", out=st2[:, 8:16], in_=sq2[:, :, 64:96], axis=AX.X, op=ALU.add); yield
                    P.A("activation", out=st2[:, 0:8], in_=st2[:, 0:8], func=AF.Sqrt, scale=1.0 / 64, bias=EPS); yield
                    P.A("activation", out=st2[:, 8:16], in_=st2[:, 8:16], func=AF.Sqrt, scale=1.0 / 32, bias=EPS); yield
                    P.V("reciprocal", out=st2[:], in_=st2[:]); yield
                    P.V("tensor_tensor", out=qs[:, :, 0:64], in0=qs[:, :, 0:64],
                        in1=st2[:, 0:8].unsqueeze(2).broadcast_to([128, 8, 64]), op=ALU.mult); yield
                    P.V("tensor_tensor", out=qs[:, :, 64:96], in0=qs[:, :, 64:96],
                        in1=st2[:, 8:16].unsqueeze(2).broadcast_to([128, 8, 32]), op=ALU.mult); yield
                    P.V("tensor_tensor", out=qs[:], in0=qs[:], in1=qg[:].unsqueeze(1).broadcast_to([128, 8, 96]), op=ALU.mult); yield
                    Qb = P.scr("Qb", [128, 8, 96], BF16, nbuf=4)
                    xs = P.scr("xs", [128, 8, 32], F32, nbuf=4)
                    r5 = qs[:, :, 64:96].rearrange("p h (a b k) -> p h a b k", a=2, b=2)
                    x5 = xs[:].rearrange("p h (a b k) -> p h a b k", a=2, b=2)
                    P.G("tensor_copy", out=x5[:, :, :, 0, :], in_=r5[:, :, :, 1, :]); yield
                    P.G("tensor_copy", out=x5[:, :, :, 1, :], in_=r5[:, :, :, 0, :]); yield
                    P.G("tensor_tensor", out=xs[:], in0=xs[:], in1=rtt[:, 1:2, :].broadcast_to([128, 8, 32]), op=ALU.mult); yield
                    P.V("tensor_tensor", out=sq2[:, :, 0:32], in0=qs[:, :, 64:96], in1=rtt[:, 0:1, :].broadcast_to([128, 8, 32]), op=ALU.mult); yield
                    P.V("tensor_tensor", out=Qb[:, :, 64:96], in0=sq2[:, :, 0:32], in1=xs[:], op=ALU.add); yield
                    P.A("activation", out=Qb[:, :, 0:64], in_=qs[:, :, 0:64], func=AF.Identity); yield
                    for h in range(8):
                        P.T("transpose", out=ptr[0:96, h, :], in_=Qb[:, h, :], identity=self.ident[:])
                    P.V("tensor_copy", out=Qb_[:, :, js], in_=ptr[0:96, :, :], pw=True); yield

                def chain_kv(j=j, js=js, ks=ks, kr=kr, rtt=rtt):
                    P.G("tensor_copy", out=Vb_[:, :, j, 0:64], in_=ks[:, :, 64:128]); yield
                    sq3 = P.scr("sq3", [128, 8, 64], F32, nbuf=4)
                    st3 = P.scr("st3", [128, 8], F32, nbuf=4)
                    P.G("tensor_tensor", out=sq3[:], in0=ks[:, :, 0:64], in1=ks[:, :, 0:64], op=ALU.mult); yield
                    P.V("tensor_reduce", out=st3[:], in_=sq3[:], axis=AX.X, op=ALU.add); yield
                    P.A("activation", out=st3[:], in_=st3[:], func=AF.Sqrt, scale=1.0 / 64, bias=EPS); yield
                    P.V("reciprocal", out=st3[:], in_=st3[:]); yield
                    Kb = P.scr("Kb", [128, 8, 96], BF16, nbuf=4)
                    P.V("tensor_tensor", out=sq3[:], in0=ks[:, :, 0:64], in1=st3[:].unsqueeze(2).broadcast_to([128, 8, 64]), op=ALU.mult); yield
                    P.V("tensor_tensor", out=Kb[:, :, 0:64], in0=sq3[:], in1=kg[:, 0:64].unsqueeze(1).broadcast_to([128, 8, 64]), op=ALU.mult); yield
                    kxs = P.scr("kxs", [128, 32], F32, nbuf=4)
                    kr2 = P.scr("kr2", [128, 32], F32, nbuf=4)
                    P.V("tensor_tensor", out=kr[:], in0=kr[:], in1=kg[:, 64:96], op=ALU.mult); yield
                    k4 = kr[:].rearrange("p (a b k) -> p a b k", a=2, b=2)
                    kx4 = kxs[:].rearrange("p (a b k) -> p a b k", a=2, b=2)
                    P.G("tensor_copy", out=kx4[:, :, 0, :], in_=k4[:, :, 1, :]); yield
                    P.G("tensor_copy", out=kx4[:, :, 1, :], in_=k4[:, :, 0, :]); yield
                    P.G("tensor_tensor", out=kxs[:], in0=kxs[:], in1=rtt[:, 1, :], op=ALU.mult); yield
                    P.V("tensor_tensor", out=kr2[:], in0=kr[:], in1=rtt[:, 0, :], op=ALU.mult); yield
                    P.V("tensor_tensor", out=kr2[:], in0=kr2[:], in1=kxs[:], op=ALU.add); yield
                    P.V("tensor_copy", out=Kb[:, :, 64:96], in_=kr2[:].unsqueeze(1).broadcast_to([128, 8, 32])); yield
                    for h in range(8):
                        P.T("transpose", out=ptr2[0:96, h, :], in_=Kb[:, h, :], identity=self.ident[:])
                    P.A("activation", out=Kb_[:, :, js], in_=ptr2[0:96, :, :], func=AF.Identity, pw=True); yield

                if need_q:
                    chains.append(chain_q())
                chains.append(chain_kv())
            if bi + 1 < len(blks_all):
                chains += hT_gens(bi + 1)
            self.run_chains(chains)
            if stop <= 6:
                continue
            if not (l == 1 and t0 >= 32):
                P.dma("sync", out=qx.ap()[:, :, t0 * 128:t0 * 128 + N], in_=Qb_[:, :, 0:N])
            for c in range(4):
                P.dma("sync", out=kxo[c].ap()[:, :, t0 * 128:t0 * 128 + N].rearrange("h d t -> d h t"), in_=Kb_[:, 2 * c:2 * c + 2, 0:N])
                P.dma("sync", out=vxo[c].ap()[:, :, t0:t0 + nt, :].rearrange("h p t e -> p h t e"), in_=Vb_[:, 2 * c:2 * c + 2, 0:nt, :])
        npt = 32 if l == 1 else NT
        P.dma("sync", out=pall_d.ap()[:, 0:npt, :], in_=p_all[:, 0:npt, :])
        P.dma("sync", out=pe.ap()[0:8, :], in_=p_all[0:8, 0, :])
        P.dma("sync", out=pe.ap()[8:16, :], in_=p_all[120:128, 31, :])
        P.end_phase()

    def phase_exchange(self, l, own_phase=True):
        P = self.P
        if own_phase:
            P.begin_phase()
        rg = [[0, 1], [2, 3], [4, 5], [6, 7]]
        q = "gpsimd"
        names = []
        for c in range(4):
            names += [f"kx_own{l}_{c}", f"vx_own{l}_{c}"]
        names.append(f"pe_own{l}")
        for nm in names:
            src = P.drams[nm]
            shp = list(src.ap().shape)
            dst = P.dram(nm.replace("_own", "_all"), [2] + shp, BF16, "Internal")
            sap, dap = src.ap(), dst.ap()
            if len(shp) == 3:
                sap = sap.rearrange("a b c -> (a b) c")
                dap = dap.rearrange("r a b c -> (r a b) c")
            elif len(shp) == 4:
                sap = sap.rearrange("a b c d -> (a b) (c d)")
                dap = dap.rearrange("r a b c d -> (r a b) (c d)")
            else:
                dap = dap.rearrange("r a b -> (r a) b")
            if P.cccount:
                P._wait(q, (P.ccsem, P.cccount))
            P._deps(q, [src], [dst], False)
            P.cccount += 1
            ev = (P.ccsem, P.cccount)
            P.ops[q].append(("cc", sap.opt(), dap.opt(), P.ccsem, rg))
            P._commit(ev, [src], [dst], False)
        if own_phase:
            P.end_phase()

    def phase_attn(self, l, with_ctx):
        P = self.P
        self.mixw = None
        if self.mode == "fused":
            P.span_begin()
            self.mixw = dict(
                wuv=P.sb_span("wuv", [128, 8, 512], BF16), wg=P.sb_span("wg", [128, 8, 3 * D], BF16),
                wbp=P.sb_span("wbp", [64, 4, D], BF16), wbs=P.sb_span("wbs", [64, 4, D], BF16),
                wbm=P.sb_span("wbm", [64, 8, D], BF16))
        P.begin_phase()
        if self.mode == "fused":
            self.phase_exchange(l, own_phase=False)
            self.load_mix_weights(l, self.mixw)
        qx = self.S(f"qx{l}", [96, 8, NTOK], BF16, False)
        kxas = [self.S(f"kx_all{l}_{c}", [2, 2, 96, NTOK], BF16, False) for c in range(4)]
        vxas = [self.S(f"vx_all{l}_{c}", [2, 2, 128, NT, 65], BF16, False) for c in range(4)]
        yatt = self.S(f"yatt{l}", [64, 8, NTOK], BF16, True)
        NKT = 66
        Kh = [P.sb(f"Kh{i}", [96, NKT * 128], BF16) for i in range(2)]
        Vh = [P.sb(f"Vh{i}", [128, NKT, 65], BF16) for i in range(2)]
        Qh = [P.sb(f"Qh{i}", [96, NTOK], BF16) for i in range(2)]
        NPS = 3
        pS = [P.ps(f"pS{i}", [128, 2, 512], F32) for i in range(NPS)]
        pacc = [P.ps(f"pacc{i}", [128, 512], F32) for i in range(2)]
        NPB = 3
        Pb = [P.sb(f"Pb{i}", [128, 2, 512], BF16) for i in range(NPB)]
        osb = [P.sb(f"osb{i}", [65, 512], F32) for i in range(2)]
        rc = [P.sb(f"rc{i}", [65, 512], F32) for i in range(2)]
        rbc = [P.sb(f"rbc{i}", [64, 512], F32) for i in range(2)]
        yb = [P.sb(f"yb{i}", [64, 512], BF16) for i in range(2)]
        rden = [P.dram(f"rden{l}_{i}", [1, 512], F32, "Internal") for i in range(2)]
        LOOK = 2
        iters = []
        heads_first_iter = {}
        qslot = 0
        for h in range(8):
            qblocks = [(i * 512, 512, list(range(NKT))) for i in range(8)]
            if with_ctx:
                qblocks.append((NLAT, 256, [32, 33]))
            heads_first_iter[h] = len(iters)
            for (q0, qn, kts) in qblocks:
                assert len(kts) % 2 == 0
                for ki in range(0, len(kts), 2):
                    assert kts[ki + 1] == kts[ki] + 1
                    iters.append((h, qslot, q0, qn, kts[ki], ki == 0, ki == len(kts) - 2))
                qslot += 1
        loaded = set()

        def load_head(h):
            if h in loaded or h >= 8:
                return
            loaded.add(h)
            K_, V_, Q_ = Kh[h % 2], Vh[h % 2], Qh[h % 2]
            kxa, vxa, hh = kxas[h // 2], vxas[h // 2], h % 2
            P.dma("sync", out=K_[:, 0:NTOK], in_=kxa.ap()[0, hh, :, :])
            P.dma("sync", out=K_[:, NTOK:NTOK + NLAT], in_=kxa.ap()[1, hh, :, 0:NLAT])
            P.dma("sync", out=V_[:, 0:NT, :], in_=vxa.ap()[0, hh, :, :, :])
            P.dma("sync", out=V_[:, NT:NKT, :], in_=vxa.ap()[1, hh, :, 0:32, :])
            P.dma("sync", out=Q_[:], in_=qx.ap()[:, h, :])

        def emit_S(i):
            h, qs, q0, qn, kt, first, last = iters[i]
            load_head(h)
            for half in range(2):
                P.T("matmul", out=pS[i % NPS][:, half, 0:qn], lhsT=Kh[h % 2][:, (kt + half) * 128:(kt + half + 1) * 128],
                    rhs=Qh[h % 2][:, q0:q0 + qn], start=True, stop=True)

        pending = []

        def make_epilogue(h, qs, q0, qn):
            acc = pacc[qs % 2]
            o_, r_, y_ = osb[qs % 2], rc[qs % 2], yb[qs % 2]

            def part1():
                P.V("tensor_copy", out=o_[:, 0:qn], in_=acc[0:65, 0:qn])
                P.V("reciprocal", out=r_[64:65, 0:qn], in_=o_[64:65, 0:qn])

            def part2():
                rd, rb_ = rden[qs % 2], rbc[qs % 2]
                P.dma("sync", out=rd.ap()[:, 0:qn], in_=r_[64:65, 0:qn])
                P.dma("sync", out=rb_[:, 0:qn], in_=rd.ap()[0, 0:qn].partition_broadcast(64))
                P.V("tensor_tensor", out=y_[:, 0:qn], in0=o_[0:64, 0:qn], in1=rb_[:, 0:qn], op=ALU.mult)
                P.dma("sync", out=yatt.ap()[:, h, q0:q0 + qn], in_=y_[:, 0:qn])
            return part1, part2

        n_it = len(iters)
        for i in range(min(LOOK, n_it)):
            emit_S(i)
        for i in range(n_it):
            h, qs, q0, qn, kt, first, last = iters[i]
            P.A("activation", out=Pb[i % NPB][:, :, 0:qn], in_=pS[i % NPS][:, :, 0:qn], func=AF.Exp, scale=SM_SCALE)
            if i + LOOK < n_it:
                emit_S(i + LOOK)
            for half in range(2):
                P.T("matmul", out=pacc[qs % 2][0:65, 0:qn], lhsT=Vh[h % 2][:, kt + half, :], rhs=Pb[i % NPB][:, half, 0:qn],
                    start=(first and half == 0), stop=(last and half == 1))
            if last:
                p1, p2 = make_epilogue(h, qs, q0, qn)
                p1()
                pending.append((i + 1, p2))
            if i == heads_first_iter[h] + 4:
                load_head(h + 1)
            while pending and pending[0][0] <= i:
                pending.pop(0)[1]()
        for _, fn in pending:
            fn()
        P.end_phase()

    def load_mix_weights(self, l, w):
        P = self.P
        win = self.w_in.ap()[l].rearrange("(c p) n -> p c n", p=128)
        P.dma("gpsimd", out=w["wuv"][:], in_=win[:, :, OFF_U:OFF_QA])
        P.newgen(w["wg"])
        for c in range(8):
            P.dma("gpsimd", out=w["wg"][:, c, :], in_=win[:, c, OFF_GATE:IN_W], pw=True)
        P.dma("gpsimd", out=w["wbp"][:], in_=self.w_br_pool.ap()[l].rearrange("(g c) n -> c g n", c=64))
        P.dma("gpsimd", out=w["wbs"][:], in_=self.w_br_sgu.ap()[l].rearrange("(g c) n -> c g n", c=64))
        P.dma("gpsimd", out=w["wbm"][:], in_=self.w_br_mla.ap()[l].rearrange("(g c) n -> c g n", c=64))

    def phase_mix(self, l, with_ctx):
        P = self.P
        P.begin_phase()
        xsrc = self.xsrc(l)
        pall_d = self.S(f"pall{l}", [128, NT, 256], BF16, False)
        pea = self.S(f"pe_all{l}", [2, 16, 256], BF16, False)
        yatt = self.S(f"yatt{l}", [64, 8, NTOK], BF16, False)
        mTd = self.S(f"mT{l}", [128, 8, NTOK], BF16, True)
        pT = P.ps("pT", [128, 8, 128], BF16)
        pA = [P.ps(f"pA{i}", [128, 512], F32) for i in range(5)]
        pS = P.ps("pSm", [128, 512], F32)
        pD = P.ps("pD", [128, 512], F32)
        self.ps_misc = pS
        if getattr(self, "mixw", None) is not None:
            w = self.mixw
        else:
            w = dict(wuv=P.sb("wuv", [128, 8, 512], BF16), wg=P.sb("wg", [128, 8, 3 * D], BF16),
                     wbp=P.sb("wbp", [64, 4, D], BF16), wbs=P.sb("wbs", [64, 4, D], BF16), wbm=P.sb("wbm", [64, 8, D], BF16))
            self.load_mix_weights(l, w)
        wuv, wg, wbp, wbs, wbm = w["wuv"], w["wg"], w["wbp"], w["wbs"], w["wbm"]
        pw_ = P.sb("poolw", [64, 4, 64], BF16)
        P.dma("gpsimd", out=pw_[:], in_=self.pool_w.ap()[l].rearrange("g c d -> c g d"))
        bnd = P.sb("bnd", [128, 5, 4, 128], BF16)
        bnc = P.sb("bnc", [128, 4, 4, 128], BF16)
        P.dma("gpsimd", out=bnd[:], in_=self.bands.ap().rearrange("n g s t -> s n g t"))
        P.dma("gpsimd", out=bnc[:], in_=self.bandcore.ap().rearrange("n g s t -> s n g t"))
        sng = P.sb("sng", [128, 256], F32)
        P.dma("sync", out=sng[:], in_=self.sgu_norm_g.ap()[l].partition_broadcast(128))
        sbb = P.sb("sbb", [64, 4, 128], F32)
        P.newgen(sbb)
        for g in range(4):
            P.dma("sync", out=sbb[:, g, :], in_=self.sgu_b.ap()[l, g].partition_broadcast(64), pw=True)
        psc = P.sb("psc", [64, 4], F32)
        self.load_fm(psc[:], self.pool_scale.ap()[l], 256, rows=64)
        bg = P.sb("bg", [128, 24], F32)
        self.load_fm(bg[:], self.b_gate.ap()[l], 3 * D)
        wsn = P.sb("wsn", [128, 4, 128], BF16)
        P.dma("gpsimd", out=wsn[:], in_=self.sgu_ws.ap()[l].rearrange("g i j -> i g j"))
        wsT = P.sb("wsT", [128, 4, 128], BF16)
        for g in range(4):
            P.T("transpose", out=pT[:, g, :], in_=wsn[:, g, :], identity=self.ident[:])
        P.V("tensor_copy", out=wsT[:], in_=pT[:, 0:4, :])
        p_all = P.sb("p_all", [128, NT, 256], BF16)
        npt = NT if with_ctx else 32
        P.dma("sync", out=p_all[:, 0:npt, :], in_=pall_d.ap()[:, 0:npt, :])
        pedge = P.sb("pedge", [32, 256], BF16)
        P.dma("sync", out=pedge[:], in_=pea.ap().rearrange("r a b -> (r a) b"))
        xts = [P.sb(f"xt{i}", [128, D], F32) for i in range(2)]
        sq = P.sb("sq", [128, D], BF16)
        xn = P.sb("xn", [128, D], BF16)
        hTs = [P.sb(f"hT{i}", [128, 8, 256], BF16) for i in range(2)]
        dT = P.sb("dT", [64, 4, 256], BF16)
        ypT = P.sb("ypT", [64, 4, 256], BF16)
        ysT = P.sb("ysT", [64, 4, 256], BF16)
        yaT = P.sb("yaT", [64, 8, 256], BF16)
        vn = P.sb("vn", [128, 2, 256], BF16)
        mT = P.sb("mT", [128, 8, 256], BF16)
        ti = [0]
        blks = self.blocks(l, with_ctx, 2)

        def produce_hT(bi):
            blk = blks[bi]
            hT = hTs[bi % 2]
            cnd = 1 if blk[0] >= 32 else 0
            P.newgen(hT)
            for j, tile in enumerate(blk):
                xt = xts[ti[0] % 2]
                ti[0] += 1
                P.dma("sync", out=xt[:], in_=xsrc.ap()[tile * 128:(tile + 1) * 128, :])
                st = P.scr("st", [128, 2], F32)
                self.emit_hT(xt, lambda c, j=j, hT=hT: hT[:, c, j * 128:(j + 1) * 128], self.a1, 0, cnd, (sq, st, xn, pT))

        produce_hT(0)
        for bi, blk in enumerate(blks):
            nt = len(blk)
            N = nt * 128
            t0 = blk[0]
            cnd = 1 if t0 >= 32 else 0
            hT = hTs[bi % 2]
            P.dma("sync", out=yaT[:, :, 0:N], in_=yatt.ap()[:, :, t0 * 128:t0 * 128 + N])
            P.newgen(vn)
            for j in range(nt):
                js = slice(j * 128, (j + 1) * 128)
                for c in range(8):
                    P.T("matmul", out=pS[:, 0:256], lhsT=hT[:, c, js], rhs=wuv[:, c, 256:512], start=(c == 0), stop=(c == 7))
                gv = P.scr("gv", [128, 256], F32)
                jk = P.scr("jk", [128, 256], F32)
                sv = P.scr("sv", [128, 2], F32)
                P.A("activation", out=gv[:], in_=pS[:, 0:256], func=AF.Gelu_apprx_tanh)
                P.A("activation", out=jk[:], in_=gv[:], func=AF.Square, accum_out=sv[:, 0:1])
                self.rstd(sv[:, 1:2], sv[:, 0:1], 256, 1)
                P.V("scalar_tensor_tensor", out=vn[:, j, :], in0=gv[:], scalar=sv[:, 1:2], in1=sng[:], op0=ALU.mult, op1=ALU.mult, pw=True)
            P.newgen(dT)
            for g in range(4):
                gs = slice(g * 64, (g + 1) * 64)
                for j, tile in enumerate(blk):
                    js = slice(j * 128, (j + 1) * 128)
                    if tile == 0:
                        srcs = [(pedge[0:32, gs], bnc[0:32, 2, g, :]), (p_all[:, 0, gs], bnc[:, 0, g, :]), (p_all[:, 1, gs], bnd[:, 2, g, :])]
                    elif tile == 31:
                        srcs = [(p_all[:, 30, gs], bnd[:, 1, g, :]), (p_all[:, 31, gs], bnc[:, 1, g, :]), (pedge[0:32, gs], bnc[0:32, 3, g, :])]
                    elif tile == 32:
                        srcs = [(p_all[:, 32, gs], bnd[:, 3, g, :]), (p_all[:, 33, gs], bnd[:, 2, g, :])]
                    elif tile == 33:
                        srcs = [(p_all[:, 32, gs], bnd[:, 1, g, :]), (p_all[:, 33, gs], bnd[:, 4, g, :])]
                    else:
                        srcs = [(p_all[:, tile - 1, gs], bnd[:, 1, g, :]), (p_all[:, tile, gs], bnd[:, 0, g, :]), (p_all[:, tile + 1, gs], bnd[:, 2, g, :])]
                    for si, (lh, rh) in enumerate(srcs):
                        P.T("matmul", out=pD[0:64, js], lhsT=lh, rhs=rh, start=(si == 0), stop=(si == len(srcs) - 1))
                P.V("tensor_copy", out=dT[:, g, 0:N], in_=pD[0:64, 0:N], pw=True)
            P.newgen(ypT)
            for g in range(4):
                P.T("matmul", out=pD[0:64, 0:N], lhsT=pw_[:, g, :], rhs=dT[:, g, 0:N], start=True, stop=True)
                P.V("tensor_scalar", out=ypT[:, g, 0:N], in0=pD[0:64, 0:N], scalar1=psc[:, g:g + 1], scalar2=None, op0=ALU.mult, pw=True)
            P.newgen(ysT)
            for g in range(4):
                gs = slice(g * 64, (g + 1) * 64)
                pa = pA[g % 2]
                for c in range(8):
                    P.T("matmul", out=pa[0:64, 0:N], lhsT=wuv[:, c, g * 64:(g + 1) * 64], rhs=hT[:, c, 0:N], start=(c == 0), stop=(c == 7))
                ug = P.scr("ug", [64, 256], F32)
                P.A("activation", out=ug[:, 0:N], in_=pa[0:64, 0:N], func=AF.Gelu_apprx_tanh)
                for j in range(nt):
                    P.T("matmul", out=pD[0:64, j * 128:(j + 1) * 128], lhsT=vn[:, j, gs], rhs=wsT[:, g, :], start=True, stop=True)
                mx = P.scr("mx", [64, 2, 128], F32)
                P.V("tensor_tensor", out=mx[:, 0:nt, :], in0=pD[0:64, 0:N].rearrange("p (a b) -> p a b", b=128),
                    in1=sbb[:, g:g + 1, :].broadcast_to([64, nt, 128]), op=ALU.add)
                P.V("tensor_tensor", out=ysT[:, g, 0:N], in0=mx[:, 0:nt, :].rearrange("p a b -> p (a b)"), in1=ug[:, 0:N], op=ALU.mult, pw=True)
            if bi + 1 < len(blks):
                produce_hT(bi + 1)
            P.newgen(mT)
            for dc in range(8):
                ds_ = slice(dc * 128, (dc + 1) * 128)
                gts = []
                for bi3 in range(3):
                    gc = bi3 * 8 + dc
                    pa = pA[bi3]
                    for c in range(8):
                        P.T("matmul", out=pa[:, 0:N], lhsT=wg[:, c, gc * 128:(gc + 1) * 128], rhs=hT[:, c, 0:N], start=(c == 0), stop=(c == 7))
                    gt = P.scr(f"gt{bi3}", [128, 256], BF16)
                    P.A("activation", out=gt[:, 0:N], in_=pa[:, 0:N], func=AF.Sigmoid, bias=bg[:, gc:gc + 1])
                    gts.append(gt)
                macc = P.scr("macc", [128, 256], F32, nbuf=1)
                tmpb = P.scr("tmpb", [128, 256], F32, nbuf=1)
                tmpc = P.scr("tmpc", [128, 256], F32, nbuf=1)
                pa = pA[3]
                for g in range(4):
                    P.T("matmul", out=pa[:, 0:N], lhsT=wbp[:, g, ds_], rhs=ypT[:, g, 0:N], start=(g == 0), stop=(g == 3))
                P.V("tensor_tensor", out=macc[:, 0:N], in0=pa[:, 0:N], in1=gts[0][:, 0:N], op=ALU.mult)
                pa = pA[4]
                for g in range(4):
                    P.T("matmul", out=pa[:, 0:N], lhsT=wbs[:, g, ds_], rhs=ysT[:, g, 0:N], start=(g == 0), stop=(g == 3))
                P.V("tensor_tensor", out=tmpb[:, 0:N], in0=pa[:, 0:N], in1=gts[1][:, 0:N], op=ALU.mult)
                P.G("tensor_tensor", out=macc[:, 0:N], in0=macc[:, 0:N], in1=tmpb[:, 0:N], op=ALU.add)
                pa = pA[3]
                for hh in range(8):
                    P.T("matmul", out=pa[:, 0:N], lhsT=wbm[:, hh, ds_], rhs=yaT[:, hh, 0:N], start=(hh == 0), stop=(hh == 7))
                P.V("tensor_tensor", out=tmpc[:, 0:N], in0=pa[:, 0:N], in1=gts[2][:, 0:N], op=ALU.mult)
                P.G("tensor_tensor", out=mT[:, dc, 0:N], in0=macc[:, 0:N], in1=tmpc[:, 0:N], op=ALU.add, pw=True)
            P.dma("sync", out=mTd.ap()[:, :, t0 * 128:t0 * 128 + N], in_=mT[:, :, 0:N])
        P.end_phase()
        if getattr(self, "mixw", None) is not None:
            P.span_end()
            self.mixw = None

    def phase_out(self, l, with_ctx):
        P = self.P
        self.moew = None
        if self.mode == "fused":
            P.span_begin()
            self.moew = dict(
                wgs=[P.sb_span(f"weg{i}", [128, 8, 512], BF16) for i in range(2)],
                wus=[P.sb_span(f"weu{i}", [128, 8, 512], BF16) for i in range(2)],
                wds=[P.sb_span(f"wed{i}", [128, 4, D], BF16) for i in range(2)])
        P.begin_phase()
        xsrc = self.xsrc(l)
        mTd = self.S(f"mT{l}", [128, 8, NTOK], BF16, False)
        xmid = self.S(f"xmid{l}", [NTOK, D], F32, True)
        h2d = self.S(f"h2T{l}", [128, 8, NTOK], BF16, True)
        pTs = [P.ps(f"pT{i}", [128, 8, 128], BF16) for i in range(2)]
        pB = [P.ps(f"pB{i}", [128, 1024], F32) for i in range(2)]
        pRs = [P.ps(f"pR{i}", [128, 512], F32) for i in range(2)]
        wo = P.sb("wo", [128, 8, D], BF16)
        P.dma("gpsimd", out=wo[:], in_=self.w_out.ap()[l].rearrange("(c p) n -> p c n", p=128))
        rw = P.sb("rw", [128, 8, NE], BF16)
        P.dma("gpsimd", out=rw[:], in_=self.router_w.ap().rearrange("(c p) n -> p c n", p=128))
        rb = P.sb("rb", [128, NE], F32)
        P.dma("sync", out=rb[:], in_=self.router_b.ap().partition_broadcast(128))
        if self.moew is not None:
            for e in range(2):
                P.dma("gpsimd", out=self.moew["wgs"][e][:], in_=self.w_e_gate.ap()[l, e].rearrange("(c p) n -> p c n", p=128))
                P.dma("gpsimd", out=self.moew["wus"][e][:], in_=self.w_e_up.ap()[l, e].rearrange("(c p) n -> p c n", p=128))
                P.dma("gpsimd", out=self.moew["wds"][e][:], in_=self.w_e_down.ap()[l, e].rearrange("(c p) n -> p c n", p=128))
        xts = [P.sb(f"xt{i}", [128, D], F32) for i in range(4)]
        xnews = [P.sb(f"xnew{i}", [128, D], F32) for i in range(4)]
        sqs = [P.sb(f"sq{i}", [128, D], BF16) for i in range(4)]
        xns = [P.sb(f"xn{i}", [128, D], BF16) for i in range(4)]
        mTs = [P.sb(f"mT{i}", [128, 8, 512], BF16) for i in range(2)]
        h2Ts = [P.sb(f"h2T{i}", [128, 8, 512], BF16) for i in range(2)]
        for bi, blk in enumerate(self.blocks(l, with_ctx)):
            nt = len(blk)
            N = nt * 128
            t0 = blk[0]
            cnd = 1 if t0 >= 32 else 0
            mT = mTs[bi % 2]
            h2T = h2Ts[bi % 2]
            pR = pRs[bi % 2]
            P.dma("sync", out=mT[:, :, 0:N], in_=mTd.ap()[:, :, t0 * 128:t0 * 128 + N])
            P.newgen(h2T)

            def sub(j, tile):
                js = slice(j * 128, (j + 1) * 128)
                pb = pB[j % 2]
                xt, xnew = xts[j], xnews[j]
                P.dma("sync", out=xt[:], in_=xsrc.ap()[tile * 128:(tile + 1) * 128, :])
                for half in range(2):
                    for dc in range(8):
                        P.T("matmul", out=pb[:, half * 512:(half + 1) * 512], lhsT=mT[:, dc, js], rhs=wo[:, dc, half * 512:(half + 1) * 512],
                            start=(dc == 0), stop=(dc == 7))
                P.V("tensor_tensor", out=xnew[:], in0=pb[:], in1=self.gbc[:, 0, cnd, :], op=ALU.mult)
                yield
                P.G("tensor_tensor", out=xnew[:], in0=xnew[:], in1=xt[:], op=ALU.add)
                yield
                P.dma("sync", out=xmid.ap()[tile * 128:(tile + 1) * 128, :], in_=xnew[:])
                st = P.scr("st", [128, 2], F32, nbuf=4)
                yield from self.gen_hT(xnew, lambda c, j=j, h2T=h2T: h2T[:, c, j * 128:(j + 1) * 128], self.a2, 24, cnd,
                                       (sqs[j], st, xns[j], pTs[j % 2]))
                for c in range(8):
                    P.T("matmul", out=pR[:, j * NE:(j + 1) * NE], lhsT=h2T[:, c, js], rhs=rw[:, c, :], start=(c == 0), stop=(c == 7))
                yield

            self.run_chains([sub(j, tile) for j, tile in enumerate(blk)])
            self.router_batch(pR[:, 0:nt * NE], rb, t0, nt)
            P.dma("sync", out=h2d.ap()[:, :, t0 * 128:t0 * 128 + N], in_=h2T[:, :, 0:N])
        P.end_phase()

    def router_batch(self, logits, rb, t0, S):
        P = self.P
        BIG = 1.0e4
        sc = P.scr("rb_sc", [128, 4, NE], F32)
        sel = P.scr("rb_sel", [128, 4, NE], F32)
        eq = P.scr("rb_eq", [128, 4, NE], F32)
        s2 = P.scr("rb_s2", [128, 4, NE], F32)
        selm = P.scr("rb_selm", [128, 4, NE], F32)
        e1 = P.scr("rb_e1", [128, 4, NE], F32)
        e2 = P.scr("rb_e2", [128, 4, NE], F32)
        m1 = P.scr("rb_m1", [128, 16], F32)
        m2 = P.scr("rb_m2", [128, 16], F32)
        ing = P.scr("rb_ing", [128, 16], F32)
        gm = P.scr("rb_gm", [128, 4, 4], F32)
        fl = lambda t: t[:, 0:S, :].rearrange("p s e -> p (s e)")
        g4 = lambda t: t[:, 0:S, :].rearrange("p s (g e) -> p (s g) e", e=4)
        P.A("activation", out=fl(sc), in_=logits, func=AF.Sigmoid)
        P.V("tensor_tensor", out=sel[:, 0:S, :], in0=sc[:, 0:S, :], in1=rb[:].unsqueeze(1).broadcast_to([128, S, NE]), op=ALU.add)
        P.V("tensor_reduce", out=m1[:, 0:S * 4], in_=g4(sel), axis=AX.X, op=ALU.max)
        P.V("tensor_tensor", out=g4(eq), in0=g4(sel), in1=m1[:, 0:S * 4].unsqueeze(2).broadcast_to([128, S * 4, 4]), op=ALU.is_equal)
        P.V("scalar_tensor_tensor", out=fl(s2), in0=fl(eq), scalar=-BIG, in1=fl(sel), op0=ALU.mult, op1=ALU.add)
        P.V("tensor_reduce", out=m2[:, 0:S * 4], in_=g4(s2), axis=AX.X, op=ALU.max)
        P.V("tensor_tensor", out=m1[:, 0:S * 4], in0=m1[:, 0:S * 4], in1=m2[:, 0:S * 4], op=ALU.add)
        grp = m1[:, 0:S * 4].rearrange("p (s g) -> p s g", g=4)
        P.V("tensor_reduce", out=gm[:, 0, 0:S], in_=grp, axis=AX.X, op=ALU.max)
        P.V("tensor_tensor", out=ing[:, 0:S * 4].rearrange("p (s g) -> p s g", g=4), in0=grp,
            in1=gm[:, 0, 0:S].unsqueeze(2).broadcast_to([128, S, 4]), op=ALU.is_equal)
        P.V("tensor_scalar", out=ing[:, 0:S * 4], in0=ing[:, 0:S * 4], scalar1=-1.0, scalar2=BIG, op0=ALU.add, op1=ALU.mult)
        P.V("tensor_tensor", out=g4(selm), in0=g4(sel), in1=ing[:, 0:S * 4].unsqueeze(2).broadcast_to([128, S * 4, 4]), op=ALU.add)
        P.V("tensor_reduce", out=gm[:, 1, 0:S], in_=selm[:, 0:S, :], axis=AX.X, op=ALU.max)
        P.V("tensor_tensor", out=e1[:, 0:S, :], in0=selm[:, 0:S, :], in1=gm[:, 1, 0:S].unsqueeze(2).broadcast_to([128, S, NE]), op=ALU.is_equal)
        P.V("scalar_tensor_tensor", out=fl(s2), in0=fl(e1), scalar=-BIG, in1=fl(selm), op0=ALU.mult, op1=ALU.add)
        P.V("tensor_reduce", out=gm[:, 2, 0:S], in_=s2[:, 0:S, :], axis=AX.X, op=ALU.max)
        P.V("tensor_tensor", out=e2[:, 0:S, :], in0=s2[:, 0:S, :], in1=gm[:, 2, 0:S].unsqueeze(2).broadcast_to([128, S, NE]), op=ALU.is_equal)
        P.V("tensor_tensor", out=fl(e1), in0=fl(e1), in1=fl(e2), op=ALU.add)
        P.V("tensor_tensor", out=fl(e1), in0=fl(e1), in1=fl(sc), op=ALU.mult)
        P.V("tensor_reduce", out=gm[:, 3, 0:S], in_=e1[:, 0:S, :], axis=AX.X, op=ALU.add)
        P.V("reciprocal", out=gm[:, 3, 0:S], in_=gm[:, 3, 0:S])
        P.V("tensor_tensor", out=self.gate_all[:, t0:t0 + S, :], in0=e1[:, 0:S, :],
            in1=gm[:, 3, 0:S].unsqueeze(2).broadcast_to([128, S, NE]), op=ALU.mult, pw=True)

    def router(self, logits, rb, tile):
        P = self.P
        BIG = 1.0e4
        sc = P.scr("r_sc", [128, NE], F32)
        sel = P.scr("r_sel", [128, 4, 4], F32)
        m1 = P.scr("r_m1", [128, 4], F32)
        eq = P.scr("r_eq", [128, 4, 4], F32)
        s2 = P.scr("r_s2", [128, 4, 4], F32)
        m2 = P.scr("r_m2", [128, 4], F32)
        gm = P.scr("r_gm", [128, 2], F32)
        ing = P.scr("r_ing", [128, 4], F32)
        e1 = P.scr("r_e1", [128, NE], F32)
        e2 = P.scr("r_e2", [128, NE], F32)
        selm = P.scr("r_selm", [128, 4, 4], F32)
        sel_f = sel[:].rearrange("p a b -> p (a b)")
        selm_f = selm[:].rearrange("p a b -> p (a b)")
        s2_f = s2[:].rearrange("p a b -> p (a b)")
        P.A("activation", out=sc[:], in_=logits, func=AF.Sigmoid)
        P.V("tensor_tensor", out=sel_f, in0=sc[:], in1=rb[:], op=ALU.add)
        P.V("tensor_reduce", out=m1[:], in_=sel[:], axis=AX.X, op=ALU.max)
        P.V("tensor_tensor", out=eq[:], in0=sel[:], in1=m1[:].unsqueeze(2).broadcast_to([128, 4, 4]), op=ALU.is_equal)
        P.V("scalar_tensor_tensor", out=s2[:], in0=eq[:], scalar=-BIG, in1=sel[:], op0=ALU.mult, op1=ALU.add)
        P.V("tensor_reduce", out=m2[:], in_=s2[:], axis=AX.X, op=ALU.max)
        P.V("tensor_tensor", out=m1[:], in0=m1[:], in1=m2[:], op=ALU.add)
        P.V("tensor_reduce", out=gm[:, 0:1], in_=m1[:], axis=AX.X, op=ALU.max)
        P.V("tensor_scalar", out=ing[:], in0=m1[:], scalar1=gm[:, 0:1], scalar2=None, op0=ALU.is_equal)
        P.V("tensor_scalar", out=ing[:], in0=ing[:], scalar1=-1.0, scalar2=BIG, op0=ALU.add, op1=ALU.mult)
        P.V("tensor_tensor", out=selm[:], in0=sel[:], in1=ing[:].unsqueeze(2).broadcast_to([128, 4, 4]), op=ALU.add)
        P.V("tensor_reduce", out=gm[:, 0:1], in_=selm_f, axis=AX.X, op=ALU.max)
        P.V("tensor_scalar", out=e1[:], in0=selm_f, scalar1=gm[:, 0:1], scalar2=None, op0=ALU.is_equal)
        P.V("scalar_tensor_tensor", out=s2_f, in0=e1[:], scalar=-BIG, in1=selm_f, op0=ALU.mult, op1=ALU.add)
        P.V("tensor_reduce", out=gm[:, 1:2], in_=s2_f, axis=AX.X, op=ALU.max)
        P.V("tensor_scalar", out=e2[:], in0=s2_f, scalar1=gm[:, 1:2], scalar2=None, op0=ALU.is_equal)
        P.V("tensor_tensor", out=e1[:], in0=e1[:], in1=e2[:], op=ALU.add)
        P.V("tensor_tensor", out=e1[:], in0=e1[:], in1=sc[:], op=ALU.mult)
        P.V("tensor_reduce", out=gm[:, 0:1], in_=e1[:], axis=AX.X, op=ALU.add)
        P.V("reciprocal", out=gm[:, 0:1], in_=gm[:, 0:1])
        P.V("tensor_scalar", out=self.gate_all[:, tile, :], in0=e1[:], scalar1=gm[:, 0:1], scalar2=None, op0=ALU.mult, pw=True)

    def phase_moe(self, l, with_ctx, final):
        P = self.P
        P.begin_phase()
        xmid = self.S(f"xmid{l}", [NTOK, D], F32, False)
        h2d = self.S(f"h2T{l}", [128, 8, NTOK], BF16, False)
        if final:
            xout = P.dram("out", [NLAT, D], F32, "ExternalOutput")
        else:
            xout = self.S("xres", [NTOK, D], F32, True)
        pG = [P.ps(f"pG{i}", [128, 512], F32) for i in range(2)]
        pU = [P.ps(f"pU{i}", [128, 512], F32) for i in range(2)]
        pY = [P.ps(f"pY{i}", [128, 1024], F32) for i in range(2)]
        tiles = list(range(NT if with_ctx else 32))
        groups = [g for g in (tiles[0:12], tiles[12:24], tiles[24:]) if g]
        pre = getattr(self, "moew", None)
        if pre is not None:
            wgs, wus, wds = pre["wgs"], pre["wus"], pre["wds"]
        else:
            wgs = [P.sb(f"weg{i}", [128, 8, 512], BF16) for i in range(2)]
            wus = [P.sb(f"weu{i}", [128, 8, 512], BF16) for i in range(2)]
            wds = [P.sb(f"wed{i}", [128, 4, D], BF16) for i in range(2)]
        h2 = P.sb("h2", [128, 8, 12 * 128], BF16)
        yacc = [P.sb(f"yacc{i}", [128, D], F32) for i in range(12)]
        aTs = [P.sb(f"aT{i}", [128, 4, 512], BF16) for i in range(2)]
        sgs = [P.sb(f"sg{i}", [128, 512], F32) for i in range(2)]
        xts = [P.sb(f"xm{i}", [128, D], F32) for i in range(2)]
        work = []
        for gi, hv in enumerate(groups):
            nth = len(hv)
            chunks = [list(range(i, min(i + 4, nth))) for i in range(0, nth, 4)]
            for e in range(NE):
                for ci, ch in enumerate(chunks):
                    work.append((gi, e, ch, ci == 0, ci == len(chunks) - 1))
        wslot = {}
        nload = [0]

        def load_w(gi, e):
            if (gi, e) in wslot or gi >= len(groups) or e >= NE:
                return
            sl = nload[0] % 2
            nload[0] += 1
            wslot[(gi, e)] = sl
            P.dma("gpsimd", out=wgs[sl][:], in_=self.w_e_gate.ap()[l, e].rearrange("(c p) n -> p c n", p=128))
            P.dma("gpsimd", out=wus[sl][:], in_=self.w_e_up.ap()[l, e].rearrange("(c p) n -> p c n", p=128))
            P.dma("gpsimd", out=wds[sl][:], in_=self.w_e_down.ap()[l, e].rearrange("(c p) n -> p c n", p=128))

        def nxt(gi, e):
            return (gi, e + 1) if e + 1 < NE else (gi + 1, 0)

        def emit_GU(k):
            gi, e, ch, first_ch, last_ch = work[k]
            hv = groups[gi]
            if e == 0 and first_ch:
                P.dma("sync", out=h2[:, :, 0:len(hv) * 128], in_=h2d.ap()[:, :, hv[0] * 128:(hv[0] + len(hv)) * 128])
            load_w(gi, e)
            sl = wslot[(gi, e)]
            N = len(ch) * 128
            cs = slice(ch[0] * 128, ch[0] * 128 + N)
            aT = aTs[k % 2]
            P.newgen(aT)
            for f in range(4):
                pg, pu, sg = pG[f % 2], pU[f % 2], sgs[f % 2]
                for c in range(8):
                    P.T("matmul", out=pg[:, 0:N], lhsT=wgs[sl][:, c, f * 128:(f + 1) * 128], rhs=h2[:, c, cs], start=(c == 0), stop=(c == 7))
                for c in range(8):
                    P.T("matmul", out=pu[:, 0:N], lhsT=wus[sl][:, c, f * 128:(f + 1) * 128], rhs=h2[:, c, cs], start=(c == 0), stop=(c == 7))
                P.A("activation", out=sg[:, 0:N], in_=pg[:, 0:N], func=AF.Silu)
                P.V("tensor_tensor", out=aT[:, f, 0:N], in0=pu[:, 0:N], in1=sg[:, 0:N], op=ALU.mult, pw=True)

        def emit_DOWN(k):
            gi, e, ch, first_ch, last_ch = work[k]
            hv = groups[gi]
            sl = wslot[(gi, e)]
            aT = aTs[k % 2]
            for jj, tl in enumerate(ch):
                py = pY[jj % 2]
                for half in range(2):
                    for f in range(4):
                        P.T("matmul", out=py[:, half * 512:(half + 1) * 512], lhsT=aT[:, f, jj * 128:(jj + 1) * 128],
                            rhs=wds[sl][:, f, half * 512:(half + 1) * 512], start=(f == 0), stop=(f == 3))
                gcol = self.gate_all[:, hv[0] + tl, e:e + 1]
                if e == 0:
                    P.V("tensor_scalar", out=yacc[tl][:], in0=py[:], scalar1=gcol, scalar2=None, op0=ALU.mult)
                else:
                    P.V("scalar_tensor_tensor", out=yacc[tl][:], in0=py[:], scalar=gcol, in1=yacc[tl][:], op0=ALU.mult, op1=ALU.add)
                if e == NE - 1:
                    emit_residual_tile(gi, tl)

        def emit_residual_tile(gi, tl):
            hv = groups[gi]
            if True:
                tile = hv[tl]
                cnd = 1 if tile >= 32 else 0
                xt = xts[tl % 2]
                P.dma("sync", out=xt[:], in_=xmid.ap()[tile * 128:(tile + 1) * 128, :])
                xo = P.scr("xo", [128, D], F32)
                P.G("tensor_tensor", out=xo[:], in0=yacc[tl][:], in1=self.gbc[:, 1, cnd, :], op=ALU.mult)
                P.G("tensor_tensor", out=xo[:], in0=xo[:], in1=xt[:], op=ALU.add)
                P.dma("sync", out=xout.ap()[tile * 128:(tile + 1) * 128, :], in_=xo[:], is_output=(final or self.mode != "fused"))

        nw = len(work)
        if pre is not None:
            wslot[(0, 0)] = 0
            wslot[(0, 1)] = 1
            nload[0] = 2
        else:
            load_w(0, 0)
            load_w(*nxt(0, 0))
        emit_GU(0)
        for k in range(nw):
            if k + 1 < nw:
                emit_GU(k + 1)
            emit_DOWN(k)
            gi, e, ch, first_ch, last_ch = work[k]
            if last_ch:
                load_w(*nxt(*nxt(gi, e)))

        P.end_phase(final=final)
        if pre is not None:
            P.span_end()
            self.moew = None


def build_program(mode, seg):
    B = Builder(mode, seg)
    P = B.P
    if mode == "fused":
        for l in range(2):
            last = (l == 1)
            B.phase_adaln(l)
            B.phase_p1(l)
            B.phase_attn(l, not last)
            B.phase_mix(l, not last)
            B.phase_out(l, not last)
            B.phase_moe(l, not last, last)
        nc = P.finish()
        return nc, list(P.ext_in), list(P.ext_out)
    if seg == 0:
        B.phase_adaln(0)
        B.phase_p1(0)
    elif seg == 1:
        B.phase_adaln(0)
        B.phase_attn(0, True)
        B.phase_mix(0, True)
        B.phase_out(0, True)
        B.phase_moe(0, True, False)
        B.phase_adaln(1)
        B.phase_p1(1)
    else:
        B.phase_adaln(1)
        B.phase_attn(1, False)
        B.phase_mix(1, False)
        B.phase_out(1, False)
        B.phase_moe(1, False, True)
    if seg != 2:
        P.begin_phase()
        P.end_phase(final=True)
    nc = P.finish()
    return nc, list(P.ext_in), list(P.ext_out)


def _rope_tables(s):
    pos = (s * NLAT + np.arange(NLAT)).astype(np.int64)
    row = (pos // 64).astype(np.float32)
    col = (pos % 64).astype(np.float32)
    inv = (np.float32(10000.0) ** (-(np.arange(8, dtype=np.float32) * np.float32(2.0) / np.float32(16.0)))).astype(np.float32)
    ar = (row[:, None] * inv).astype(np.float32)
    ac = (col[:, None] * inv).astype(np.float32)
    ang = np.concatenate([ar, ar, ac, ac], axis=-1)
    cos = np.cos(ang).astype(np.float32)
    sin = np.sin(ang).astype(np.float32)
    sgn = np.concatenate([-np.ones(8), np.ones(8), -np.ones(8), np.ones(8)]).astype(np.float32)
    t = np.zeros((NTOK, 2, 32), np.float32)
    t[:NLAT, 0] = cos
    t[:NLAT, 1] = sin * sgn
    t[NLAT:, 0] = 1.0
    return t


def _pool_coef(t, s, w, n):
    lo = max(t - w // 2, 0)
    hi = min(t + (w - w // 2), n)
    v = 0.0
    if lo <= s < hi:
        v += 1.0 / (hi - lo)
    if s == t:
        v -= 1.0
    return v


def _band(t_base, s_base, n, rows=128):
    out = np.zeros((4, 128, 128), np.float32)
    for g, w in enumerate((2, 4, 8, 16)):
        for tl in range(128):
            t = t_base + tl
            for s in range(max(t - 9, s_base), min(t + 9, s_base + rows)):
                if 0 <= s < n:
                    out[g, s - s_base, tl] = _pool_coef(t, s, w, n)
    return out


def _bands_universal():
    n = 8192
    mid = 10 * 128
    b = np.zeros((5, 4, 128, 128), np.float32)
    b[0] = _band(mid, mid, n)
    b[1] = _band(mid, mid - 128, n)
    b[2] = _band(mid, mid + 128, n)
    b[3] = _band(0, 0, n)
    b[4] = _band(n - 128, n - 128, n)
    return b


def _bands_core(s, uni):
    n = 8192
    b = np.zeros((4, 4, 128, 128), np.float32)
    if s == 0:
        b[0] = uni[3]
        b[1] = uni[0]
        e = _band(3968, 4096, n, rows=8)
        b[3][:, 16:24, :] = e[:, 0:8, :]
    else:
        b[0] = uni[0]
        b[1] = uni[4]
        e = _band(4096, 4088, n, rows=8)
        b[2][:, 8:16, :] = e[:, 0:8, :]
    return b


_CACHE = {}
FUSED = True
_LAST_STATE = None


def _get_prog(mode, seg):
    key = (mode, seg)
    if key not in _CACHE:
        _CACHE[key] = build_program(mode, seg)
    return _CACHE[key]


def kernel(**inp):
    inp = {k: np.ascontiguousarray(np.asarray(v)) for k, v in inp.items()}
    x, c, ctx, c_ctx = inp["x"], inp["c"], inp["ctx"], inp["c_ctx"]
    uni = _bands_universal()
    wnames = ["w_mod", "b_mod", "norm1_g", "norm2_g", "w_in", "pool_w", "pool_scale", "sgu_norm_g", "sgu_ws", "sgu_b",
              "qa_norm_g", "w_uq", "kva_norm_g", "w_ukv", "q_norm_g", "k_norm_g", "w_br_pool", "w_br_sgu", "w_br_mla",
              "b_gate", "w_out", "router_w", "router_b", "w_e_gate", "w_e_up", "w_e_down"]
    base = []
    for core in range(8):
        b, s = core // 2, core % 2
        m = {k: inp[k] for k in wnames}
        m["xin"] = np.concatenate([x[b, s * NLAT:(s + 1) * NLAT], ctx[b]], axis=0)
        m["cond"] = np.stack([c[b], c_ctx], axis=0)
        m["ropet"] = _rope_tables(s)
        m["bands"] = uni
        m["bandcore"] = _bands_core(s, uni)
        base.append(m)
    if FUSED:
        nc, ext_in, ext_out = _get_prog("fused", 0)
        in_maps = [{n: base[core][n] for n in ext_in} for core in range(8)]
        res = run_bass_kernel_spmd(nc, in_maps, core_ids=list(range(8)))
        out = np.zeros((4, 8192, D), np.float32)
        for core in range(8):
            b, s = core // 2, core % 2
            out[b, s * NLAT:(s + 1) * NLAT] = np.asarray(res.results[core]["out"])
        return out
    state = [dict() for _ in range(8)]
    global _LAST_STATE
    _LAST_STATE = state
    out = None
    for seg in range(3):
        nc, ext_in, ext_out = _get_prog("seg", seg)
        in_maps = []
        for core in range(8):
            mm = {}
            for n in ext_in:
                if n in base[core]:
                    mm[n] = base[core][n]
                else:
                    mm[n] = state[core][n]
            in_maps.append(mm)
        res = run_bass_kernel_spmd(nc, in_maps, core_ids=list(range(8)))
        for core in range(8):
            for n in ext_out:
                state[core][n] = np.asarray(res.results[core][n])
        for nm in ext_out:
            if "_own" in nm:
                for pair in range(4):
                    c0, c1 = 2 * pair, 2 * pair + 1
                    allv = np.stack([state[c0][nm], state[c1][nm]], axis=0)
                    state[c0][nm.replace("_own", "_all")] = allv
                    state[c1][nm.replace("_own", "_all")] = allv
        if "out" in ext_out:
            out = np.zeros((4, 8192, D), np.float32)
            for core in range(8):
                b, s = core // 2, core % 2
                out[b, s * NLAT:(s + 1) * NLAT] = state[core]["out"]
    return out
```
